# Optimizing a Trainium2 kernel written in Bass

```python
import math
import jax, jax.numpy as jnp
from jax import lax
import numpy as np

D_MODEL = 1024
BATCH = 8
SEQ = 2048
DEPTH = 1

CHUNK = 64
A_HEADS = 8
A_HEAD_DIM = 64
A_LEFT_CHUNKS = 8
A_MAX_REL = 128
A_WIDTH = A_HEADS * A_HEAD_DIM
B_HEADS = 8
B_NOPE_DIM = 64
B_ROPE_DIM = 32
B_V_DIM = 64
B_Q_LORA = 384
B_KV_LORA = 256
B_QK_DIM = B_NOPE_DIM + B_ROPE_DIM
B_WIDTH = B_HEADS * B_V_DIM
ROPE_THETA = 10000.0
Q_BLOCK = 128
IN_SIZES = (A_WIDTH, A_WIDTH, A_WIDTH, B_Q_LORA, B_KV_LORA, B_ROPE_DIM, D_MODEL, D_MODEL)
IN_SPLITS = tuple(int(v) for v in np.cumsum(IN_SIZES)[:-1])
N_IN = int(sum(IN_SIZES))
N_EXPERTS = 32
TOP_K = 4
D_EXPERT = D_MODEL
SWIGLU_LIMIT = 7.0
SWIGLU_ALPHA = 1.702
EXPERT_BLOCK = 256
DEEPNORM_ALPHA = (2.0 * DEPTH) ** 0.25
DEEPNORM_BETA = (8.0 * DEPTH) ** -0.25
LN_EPS = 1e-5
RMS_EPS = 1e-6
NEG_INF = -1e30

kernel_name = "hybrid_chunked_mla_moe_deepnorm_adaln"


def layer_norm(x, g, b):
    xf = x.astype(jnp.float32)
    mu = jnp.mean(xf, axis=-1, keepdims=True)
    var = jnp.mean(jnp.square(xf - mu), axis=-1, keepdims=True)
    return ((xf - mu) * lax.rsqrt(var + LN_EPS) * g.astype(jnp.float32) + b.astype(jnp.float32)).astype(x.dtype)


def rms_norm(x, g):
    xf = x.astype(jnp.float32)
    ms = jnp.mean(jnp.square(xf), axis=-1, keepdims=True)
    return (xf * lax.rsqrt(ms + RMS_EPS) * g.astype(jnp.float32)).astype(x.dtype)


def rope(x, positions):
    half = x.shape[-1] // 2
    freqs = ROPE_THETA ** (-jnp.arange(half, dtype=jnp.float32) / half)
    ang = positions.astype(jnp.float32)[:, :, None, None] * freqs
    cos, sin = jnp.cos(ang), jnp.sin(ang)
    xf = x.astype(jnp.float32)
    x1, x2 = xf[..., :half], xf[..., half:]
    return jnp.concatenate([x1 * cos - x2 * sin, x2 * cos + x1 * sin], axis=-1).astype(x.dtype)


def chunked_relpos_attention(q, k, v, rel_bias):
    bsz, seq = q.shape[0], q.shape[1]
    n_chunks = seq // CHUNK
    left = A_LEFT_CHUNKS * CHUNK
    band = left + CHUNK
    qc = q.reshape(bsz, n_chunks, CHUNK, A_HEADS, A_HEAD_DIM)
    pad = ((0, 0), (left, 0), (0, 0), (0, 0))
    band_idx = (jnp.arange(n_chunks) * CHUNK)[:, None] + jnp.arange(band)[None, :]
    kb = jnp.pad(k, pad)[:, band_idx]
    vb = jnp.pad(v, pad)[:, band_idx]
    valid = band_idx >= left
    dist = jnp.arange(CHUNK)[:, None] - jnp.arange(band)[None, :] + left
    bias = rel_bias[:, jnp.clip(dist, -A_MAX_REL, A_MAX_REL) + A_MAX_REL].astype(jnp.float32)
    s = jnp.einsum('bcqhd,bckhd->bhcqk', qc, kb, preferred_element_type=jnp.float32)
    s = s * (A_HEAD_DIM ** -0.5) + bias[:, None]
    s = jnp.where(valid[None, None, :, None, :], s, NEG_INF)
    p = jax.nn.softmax(s, axis=-1).astype(v.dtype)
    o = jnp.einsum('bhcqk,bckhd->bcqhd', p, vb)
    return o.reshape(bsz, seq, A_WIDTH)


def mla_attention(c_q, c_kv, k_rope, positions, rms_q, w_uq, rms_kv, w_ukv):
    bsz, seq = c_q.shape[0], c_q.shape[1]
    q = (rms_norm(c_q, rms_q) @ w_uq).reshape(bsz, seq, B_HEADS, B_QK_DIM)
    kv = (rms_norm(c_kv, rms_kv) @ w_ukv).reshape(bsz, seq, B_HEADS, B_NOPE_DIM + B_V_DIM)
    q_nope, q_pe = q[..., :B_NOPE_DIM], q[..., B_NOPE_DIM:]
    k_nope, v = kv[..., :B_NOPE_DIM], kv[..., B_NOPE_DIM:]
    q_pe = rope(q_pe, positions)
    k_pe = rope(k_rope[:, :, None, :], positions)
    q = jnp.concatenate([q_nope, q_pe], axis=-1)
    k = jnp.concatenate([k_nope, jnp.broadcast_to(k_pe, (bsz, seq, B_HEADS, B_ROPE_DIM))], axis=-1)
    n_qb = seq // Q_BLOCK
    qb = q.reshape(bsz, n_qb, Q_BLOCK, B_HEADS, B_QK_DIM).transpose(1, 0, 2, 3, 4)
    key_chunk = jnp.arange(seq) // CHUNK
    scale = B_QK_DIM ** -0.5

    def one_block(args):
        q_blk, start = args
        q_chunk = (start + jnp.arange(Q_BLOCK)) // CHUNK
        mask = key_chunk[None, :] <= q_chunk[:, None]
        s = jnp.einsum('bqhd,bkhd->bhqk', q_blk, k, preferred_element_type=jnp.float32) * scale
        s = jnp.where(mask[None, None], s, NEG_INF)
        p = jax.nn.softmax(s, axis=-1).astype(v.dtype)
        return jnp.einsum('bhqk,bkhd->bqhd', p, v)

    o = lax.map(one_block, (qb, jnp.arange(n_qb) * Q_BLOCK))
    return o.transpose(1, 0, 2, 3, 4).reshape(bsz, seq, B_WIDTH)


def moe_ffn(u, w_router, b_router, w_gate, b_gate, w_up, b_up, w_down, b_down):
    bsz, seq, d = u.shape
    n_tok = bsz * seq
    xt = u.reshape(n_tok, d)
    logits = jnp.dot(xt, w_router, preferred_element_type=jnp.float32) + b_router.astype(jnp.float32)
    top_val, top_idx = lax.top_k(logits, TOP_K)
    top_w = jax.nn.softmax(top_val, axis=-1)
    n_slots = n_tok * TOP_K
    slot_expert = top_idx.reshape(-1).astype(jnp.int32)
    slot_token = jnp.arange(n_slots, dtype=jnp.int32) // TOP_K
    slot_w = top_w.reshape(-1)
    order = jnp.argsort(slot_expert)
    sorted_expert = slot_expert[order]
    counts = jnp.bincount(slot_expert, length=N_EXPERTS)
    padded = ((counts + EXPERT_BLOCK - 1) // EXPERT_BLOCK) * EXPERT_BLOCK
    start = jnp.cumsum(counts) - counts
    pstart = jnp.cumsum(padded) - padded
    dest = pstart[sorted_expert] + jnp.arange(n_slots, dtype=jnp.int32) - start[sorted_expert]
    n_blocks = -(-n_slots // EXPERT_BLOCK) + N_EXPERTS
    cap = n_blocks * EXPERT_BLOCK
    buf_token = jnp.zeros((cap,), jnp.int32).at[dest].set(slot_token[order])
    buf_w = jnp.zeros((cap,), jnp.float32).at[dest].set(slot_w[order])
    block_start = jnp.arange(n_blocks, dtype=jnp.int32) * EXPERT_BLOCK
    block_expert = jnp.clip(jnp.searchsorted(jnp.cumsum(padded), block_start, side='right'), 0, N_EXPERTS - 1)

    def expert_block(args):
        tok, e = args
        xb = xt[tok]
        g = xb @ w_gate[e] + b_gate[e]
        up = xb @ w_up[e] + b_up[e]
        g = jnp.minimum(g, SWIGLU_LIMIT)
        up = jnp.clip(up, -SWIGLU_LIMIT, SWIGLU_LIMIT)
        h = (up + 1.0) * (g * jax.nn.sigmoid(SWIGLU_ALPHA * g))
        return h @ w_down[e] + b_down[e]

    y = lax.map(expert_block, (buf_token.reshape(n_blocks, EXPERT_BLOCK), block_expert))
    y = y.reshape(cap, d) * buf_w[:, None].astype(y.dtype)
    out = jnp.zeros((n_tok, d), u.dtype).at[buf_token].add(y)
    return out.reshape(bsz, seq, d)


def setup_inputs(seed: int = 0) -> dict:
    key = jax.random.key(seed)
    ks = jax.random.split(key, 26)

    def nrm(k, shape, scale):
        return jax.random.normal(k, shape, jnp.float32) * scale

    L, D, E, F = DEPTH, D_MODEL, N_EXPERTS, D_EXPERT
    positions = (jax.random.randint(ks[2], (BATCH, 1), 0, 4096, dtype=jnp.int32)
                 + jnp.arange(SEQ, dtype=jnp.int32)[None, :])
    return {
        "x": nrm(ks[0], (BATCH, SEQ, D), 1.0),
        "c": nrm(ks[1], (BATCH, D), 1.0),
        "positions": positions,
        "w_ada": nrm(ks[3], (L, D, 6 * D), 0.5 * D ** -0.5),
        "b_ada": nrm(ks[4], (L, 6 * D), 0.01),
        "w_in": nrm(ks[5], (L, D, N_IN), D ** -0.5),
        "rms_q": 1.0 + nrm(ks[6], (L, B_Q_LORA), 0.01),
        "w_uq": nrm(ks[7], (L, B_Q_LORA, B_HEADS * B_QK_DIM), B_Q_LORA ** -0.5),
        "rms_kv": 1.0 + nrm(ks[8], (L, B_KV_LORA), 0.01),
        "w_ukv": nrm(ks[9], (L, B_KV_LORA, B_HEADS * (B_NOPE_DIM + B_V_DIM)), B_KV_LORA ** -0.5),
        "rel_bias": nrm(ks[10], (L, A_HEADS, 2 * A_MAX_REL + 1), 0.5),
        "w_branch_a": nrm(ks[11], (L, A_WIDTH, D), DEEPNORM_BETA * A_WIDTH ** -0.5),
        "w_branch_b": nrm(ks[12], (L, B_WIDTH, D), DEEPNORM_BETA * B_WIDTH ** -0.5),
        "w_out": nrm(ks[13], (L, D, D), DEEPNORM_BETA * D ** -0.5),
        "ln1_g": 1.0 + nrm(ks[14], (L, D), 0.01),
        "ln1_b": nrm(ks[15], (L, D), 0.01),
        "w_router": nrm(ks[16], (L, D, E), D ** -0.5),
        "b_router": nrm(ks[17], (L, E), 0.01),
        "w_gate": nrm(ks[18], (L, E, D, F), D ** -0.5),
        "b_gate": nrm(ks[19], (L, E, F), 0.01),
        "w_up": nrm(ks[20], (L, E, D, F), D ** -0.5),
        "b_up": nrm(ks[21], (L, E, F), 0.01),
        "w_down": nrm(ks[22], (L, E, F, D), DEEPNORM_BETA * F ** -0.5),
        "b_down": nrm(ks[23], (L, E, D), 0.01),
        "ln2_g": 1.0 + nrm(ks[24], (L, D), 0.01),
        "ln2_b": nrm(ks[25], (L, D), 0.01),
    }


def reference(x, c, positions, w_ada, b_ada, w_in, rms_q, w_uq, rms_kv, w_ukv, rel_bias,
              w_branch_a, w_branch_b, w_out, ln1_g, ln1_b, w_router, b_router,
              w_gate, b_gate, w_up, b_up, w_down, b_down, ln2_g, ln2_b):
    bsz, seq = x.shape[0], x.shape[1]
    for l in range(DEPTH):
        mod = jnp.einsum('bd,dm->bm', jax.nn.silu(c), w_ada[l]) + b_ada[l]
        shift1, scale1, gate1, shift2, scale2, gate2 = jnp.split(mod[:, None, :], 6, axis=-1)

        h = x * (1.0 + scale1) + shift1
        z = h @ w_in[l]
        q_a, k_a, v_a, c_q, c_kv, k_rope, g_a, g_b = jnp.split(z, IN_SPLITS, axis=-1)
        y_a = chunked_relpos_attention(
            q_a.reshape(bsz, seq, A_HEADS, A_HEAD_DIM),
            k_a.reshape(bsz, seq, A_HEADS, A_HEAD_DIM),
            v_a.reshape(bsz, seq, A_HEADS, A_HEAD_DIM),
            rel_bias[l])
        y_b = mla_attention(c_q, c_kv, k_rope, positions, rms_q[l], w_uq[l], rms_kv[l], w_ukv[l])
        merged = (jax.nn.sigmoid(g_a) * (y_a @ w_branch_a[l])
                  + jax.nn.sigmoid(g_b) * (y_b @ w_branch_b[l]))
        x = layer_norm(DEEPNORM_ALPHA * x + gate1 * (merged @ w_out[l]), ln1_g[l], ln1_b[l])

        h = x * (1.0 + scale2) + shift2
        f = moe_ffn(h, w_router[l], b_router[l], w_gate[l], b_gate[l], w_up[l], b_up[l], w_down[l], b_down[l])
        x = layer_norm(DEEPNORM_ALPHA * x + gate2 * f, ln2_g[l], ln2_b[l])
    return x
```

```python
import math
from contextlib import ExitStack

import numpy as np
import concourse.bass as bass
import concourse.mybir as mybir
from concourse.bass_utils import run_bass_kernel_spmd

F32 = mybir.dt.float32
BF16 = mybir.dt.bfloat16
I32 = mybir.dt.int32
AF = mybir.ActivationFunctionType
ALU = mybir.AluOpType
ENGS = ['pe', 'act', 'dve', 'pool', 'sp']
P5G = 0
P5K = 99
P5M = 'pad'

S = 2048
D = 1024
NT = 16
NG = 4
NE = 32
ALPHA = 2.0 ** 0.25
LN_EPS = 1e-5
RMS_EPS = 1e-6
SIG_MAX = 1.0 / (1.0 + math.exp(-1.702 * 7.0))
TWO_PI = 2.0 * math.pi
CW1 = 6.28125
CW2 = TWO_PI - 6.28125
PI_LO = 3.1415925


class Prog:
    def __init__(self, nc):
        self.nc = nc
        self.ops = []
        self.last_writer = {}
        self.readers = {}
        self.dma_cum = {}

    def op(self, eng, fn, reads=(), writes=(), dma_key=None):
        ps_reads = [k for k in reads if isinstance(k, tuple) and k[0] == 'ps']
        if ps_reads:
            reads = [k for k in reads if k not in ps_reads]
            writes = list(writes) + ps_reads
        idx = len(self.ops)
        deps = set()
        for k in reads:
            w = self.last_writer.get(k)
            if w is not None:
                deps.add(w)
        for k in writes:
            w = self.last_writer.get(k)
            if w is not None:
                deps.add(w)
            for r in self.readers.get(k, ()):
                deps.add(r)
        o = dict(eng=eng, fn=fn, deps=deps, dma_key=dma_key, signal=False)
        if dma_key is not None:
            self.dma_cum[dma_key] = self.dma_cum.get(dma_key, 0) + 16
            o['dma_cum'] = self.dma_cum[dma_key]
        self.ops.append(o)
        for k in reads:
            self.readers.setdefault(k, []).append(idx)
        for k in writes:
            self.last_writer[k] = idx
            self.readers[k] = []
        return idx

    def barrier(self):
        last = {}
        dm = {}
        for i, o in enumerate(self.ops):
            if o['fn'] is None:
                continue
            last[o['eng']] = i
            if o['dma_key'] is not None:
                dm[o['dma_key']] = i
        deps = set(last.values()) | set(dm.values())
        for e in ENGS:
            self.ops.append(dict(eng=e, fn=None, deps=set(deps), dma_key=None, signal=False))

    def emit(self, stack):
        nc = self.nc
        ops = self.ops
        for o in ops:
            for d in o['deps']:
                a = ops[d]
                if a['fn'] is None or a['dma_key'] is not None:
                    continue
                if a['eng'] == 'pe' and o['eng'] == 'pe':
                    continue
                a['signal'] = True
        cnt = {e: 0 for e in ENGS}
        for o in ops:
            if o['fn'] is not None and o['dma_key'] is None and o['signal']:
                cnt[o['eng']] += 1
                o['sig_n'] = cnt[o['eng']]
        sems = {e: stack.enter_context(nc.semaphore("S_" + e)) for e in ['pe', 'act', 'dve', 'pool', 'sp']}
        dsems = {k: stack.enter_context(nc.semaphore("D_%d" % i)) for i, k in enumerate(self.dma_cum)}

        def run(ename, eh):
            waited = {}
            for o in ops:
                if o['eng'] != ename:
                    continue
                need = {}
                for d in sorted(o['deps']):
                    a = ops[d]
                    if a['fn'] is None:
                        continue
                    if a['dma_key'] is not None:
                        key = ('D', a['dma_key'])
                        val = a['dma_cum']
                        sem = dsems[a['dma_key']]
                    else:
                        if a['eng'] == 'pe' and ename == 'pe':
                            continue
                        key = ('S', a['eng'])
                        val = a['sig_n']
                        sem = sems[a['eng']]
                    if key not in need or need[key][0] < val:
                        need[key] = (val, sem)
                for key, (val, sem) in need.items():
                    if waited.get(key, 0) >= val:
                        continue
                    waited[key] = val
                    eh.wait_ge(sem, val)
                if o['fn'] is None:
                    continue
                ins = o['fn']()
                if o['dma_key'] is not None:
                    ins.then_inc(dsems[o['dma_key']], 16)
                elif o['signal']:
                    ins.then_inc(sems[ename], 1)

        with nc.Block() as block:
            @block.tensor
            def _(e):
                run('pe', e)

            @block.scalar
            def _(e):
                run('act', e)

            @block.vector
            def _(e):
                run('dve', e)

            @block.gpsimd
            def _(e):
                run('pool', e)

            @block.sync
            def _(e):
                run('sp', e)


class Arena:
    def __init__(self, tensor, nwords):
        self.t = tensor
        self.n = nwords
        self.live = {}
        self.pending = []

    def _alloc(self, name, words, top=False):
        words = (words + 7) // 8 * 8
        spans = sorted(self.live.values())
        assert name not in self.live, name
        if top:
            pos = self.n
            for (a, b) in reversed(spans):
                if pos - b >= words:
                    break
                pos = min(pos, a)
            pos -= words
            if pos < 0 or any(a < pos + words and pos < b for (a, b) in spans):
                raise RuntimeError("arena OOM(top) for %s (%d words)" % (name, words))
        else:
            pos = 0
            for (a, b) in spans:
                if a - pos >= words:
                    break
                pos = max(pos, b)
            if pos + words > self.n:
                raise RuntimeError("arena OOM for %s (%d words) live=%s" % (name, words, sorted(self.live.items(), key=lambda kv: kv[1])))
        self.live[name] = (pos, pos + words)
        return pos

    def f32(self, name, n, top=False):
        off = self._alloc(name, n, top)
        return self.t[:, off:off + n]

    def bf16(self, name, n, top=False):
        w = (n + 1) // 2
        off = self._alloc(name, w, top)
        return self.t[:, off:off + w].bitcast(BF16)

    def i32(self, name, n):
        off = self._alloc(name, n)
        return self.t[:, off:off + n].bitcast(I32)

    def free(self, *names):
        self.pending.extend(names)

    def commit(self):
        for nme in self.pending:
            del self.live[nme]
        self.pending = []


def build_program(stage="full", n_exp=NE):
    nc = bass.Bass("TRN2", target_bir_lowering=False, dynamic_dma_scratch_size=512)

    def din(name, shape, dt=F32):
        return nc.dram_tensor(name, shape, dt, kind="ExternalInput").ap()

    x_d = din("x", [S, D])
    ccol_d = din("ccol", [128, 8])
    pos_d = din("pos", [1, S], I32)
    wada_d = din("w_ada", [D, 6 * D])
    bada_d = din("b_ada", [1, 6 * D])
    win_d = din("w_in", [D, 4256])
    rmsq_d = din("rmsq", [128, 3])
    rmskv_d = din("rmskv", [128, 2])
    wuq_d = din("w_uq", [384, 768])
    wukv_d = din("w_ukv", [256, 1024])
    biasT_d = din("biasT", [128, 8 * 5 * 128])
    maskT_d = din("maskT", [128, 5 * 128])
    wba_d = din("w_ba", [512, D])
    wbb_d = din("w_bb", [512, D])
    wout_d = din("w_out", [D, D])
    ln_d = din("ln", [4, D])
    wr_d = din("w_router", [D, NE])
    br_d = din("b_router", [1, NE])
    wg_d = wu_d = wd_d = None
    if stage not in ("x1", "p5", "p5a", "p5b", "p5c", "p5d", "p5e"):
        wg_d = din("w_gate", [n_exp, D, D])
        wu_d = din("w_up", [n_exp, D, D])
        wd_d = din("w_down", [n_exp, D, D])
    bgcol_d = din("bgcol", [128, NE * 8])
    bucol_d = din("bucol", [128, NE * 8])
    bdown_d = din("b_down", [NE, D])
    ident_d = din("ident", [128, 128])
    freq_d = din("freq", [128, 1])
    out_d = nc.dram_tensor("out", [S, D], F32, kind="ExternalOutput").ap()

    P = Prog(nc)
    NAR = 55700
    st = ExitStack()
    with st:
        arena_t = st.enter_context(nc.sbuf_tensor("arena", [128, NAR], F32))
        PS = st.enter_context(nc.psum_tensor("PS", [128, 8, 512], F32))
        A = Arena(arena_t, NAR)
        V = nc.vector
        G = nc.gpsimd
        ACT = nc.scalar
        PE = nc.tensor

        bank_rr = [0]

        def nb(lo=0, hi=8):
            b = lo + bank_rr[0] % (hi - lo)
            bank_rr[0] += 1
            return b

        def dma(out, in_, key, reads=(), writes=(), q='sp'):
            if q == 'sp':
                P.op('sp', lambda: nc.sync.dma_start(out=out, in_=in_), reads=list(reads), writes=list(writes), dma_key=key)
            else:
                P.op('act', lambda: nc.scalar.dma_start(out=out, in_=in_), reads=list(reads), writes=list(writes), dma_key=key)

        def phase_end(*free_names):
            A.free(*free_names)
            P.barrier()
            A.commit()

        ident = A.f32('ident', 128)
        dma(ident, ident_d, 'ident', writes=['ident'])
        identb = A.bf16('identb', 128)
        P.op('dve', lambda: V.tensor_copy(out=identb, in_=ident), reads=['ident'], writes=['identb'])
        ones_bf = A.bf16('ones_bf', 128)
        P.op('pool', lambda: G.memset(ones_bf, 1.0), writes=['ones_bf'])
        ones_f = A.f32('ones_f', 128)
        P.op('pool', lambda: G.memset(ones_f, 1.0), writes=['ones_f'])
        modcol = A.f32('modcol', 96)
        onep = A.f32('onep', 16)
        g1bc = A.f32('g1bc', D)
        g2bc = A.f32('g2bc', D)
        ccol = A.f32('ccol', 8)
        sc = A.f32('sc', 8)
        dma(ccol, ccol_d, 'ccol', writes=['ccol'])
        sgc = A.f32('sgc', 8)
        P.op('act', lambda: ACT.activation(out=sgc, in_=ccol, func=AF.Sigmoid), reads=['ccol'], writes=['sgc'])
        P.op('dve', lambda: V.tensor_tensor(out=sc, in0=ccol, in1=sgc, op=ALU.mult), reads=['ccol', 'sgc'], writes=['sc'])

        def col(j):
            return modcol[:, 2 * j:2 * j + 1]

        h1T = A.bf16('h1T', 8 * S).rearrange("p (k t) -> p k t", k=8)
        xs = [A.f32('xs%d' % i, 4 * D).rearrange("p (t f) -> p t f", t=4) for i in range(2)]
        for tg in range(2):
            dma(xs[tg], x_d[tg * 512:(tg + 1) * 512, :].rearrange("(t p) f -> p t f", p=128), ('xs', tg), writes=[('xs', tg)],
                q=('sp' if tg % 2 == 0 else 'act'))
        wst = [A.f32('wst%d' % i, 4096) for i in range(4)]
        modrow = A.f32('modrow', 6 * D)
        badar = A.f32('badar', 6 * D)
        dma(badar[0:1, :], bada_d, 'badar', writes=['badar'])
        for n in range(12):
            s = n % 4
            dma(wst[s].rearrange("p (k f) -> p k f", k=8),
                wada_d[:, n * 512:(n + 1) * 512].rearrange("(k p) f -> p k f", p=128),
                ('wst', s), writes=[('wst', s)], q=('sp' if n % 2 == 0 else 'act'))
            b = nb()

            def f(s=s, b=b):
                for kc in range(8):
                    ins = PE.matmul(PS[0:1, b, :], lhsT=sc[:, kc:kc + 1], rhs=wst[s][:, kc * 512:(kc + 1) * 512],
                                    start=(kc == 0), stop=(kc == 7))
                return ins
            P.op('pe', f, reads=['sc', ('wst', s)], writes=[('ps', b)])
            P.op('dve', lambda n=n, b=b: V.tensor_tensor(out=modrow[0:1, n * 512:(n + 1) * 512], in0=PS[0:1, b, :],
                                                         in1=badar[0:1, n * 512:(n + 1) * 512], op=ALU.add),
                 reads=[('ps', b), 'badar'], writes=['modrow'])
        b = nb()

        def f(b=b):
            for j in range(48):
                ins = PE.matmul(PS[:, b, 2 * j:2 * j + 2], lhsT=modrow[0:1, j * 128:(j + 1) * 128], rhs=ones_f[0:1, 0:2],
                                start=True, stop=True)
            return ins
        P.op('pe', f, reads=['modrow', 'ones_f'], writes=[('ps', b)])
        P.op('dve', lambda b=b: V.tensor_copy(out=modcol, in_=PS[:, b, 0:96]), reads=[('ps', b)], writes=['modcol'])
        P.op('dve', lambda: V.tensor_scalar(out=onep[:, 0:8], in0=modcol[:, 16:32:2], scalar1=1.0, scalar2=None, op0=ALU.add),
             reads=['modcol'], writes=['onep'])
        P.op('dve', lambda: V.tensor_scalar(out=onep[:, 8:16], in0=modcol[:, 64:80:2], scalar1=1.0, scalar2=None, op0=ALU.add),
             reads=['modcol', 'onep'], writes=['onep'])
        for (gb, base) in ((g1bc, 2 * D), (g2bc, 5 * D)):
            for hh in range(2):
                b = nb()
                P.op('pe', lambda b=b, base=base, hh=hh: PE.matmul(PS[:, b, :], lhsT=ones_f[0:1, :],
                                                                   rhs=modrow[0:1, base + hh * 512:base + (hh + 1) * 512],
                                                                   start=True, stop=True),
                     reads=['modrow', 'ones_f'], writes=[('ps', b)])
                P.op('act', lambda b=b, gb=gb, hh=hh: ACT.copy(out=gb[:, hh * 512:(hh + 1) * 512], in_=PS[:, b, :]),
                     reads=[('ps', b)], writes=[('gbc', id(gb), hh)])
        A.free('wst0', 'wst1', 'wst2', 'wst3', 'modrow', 'badar', 'ccol', 'sc', 'sgc')

        for tg in range(NG):
            s = tg % 2
            if tg >= 2:
                dma(xs[s], x_d[tg * 512:(tg + 1) * 512, :].rearrange("(t p) f -> p t f", p=128), ('xs', s), writes=[('xs', s)],
                    q=('sp' if tg % 2 == 0 else 'act'))
            for kc in range(8):
                b = nb()

                def f(s=s, kc=kc, b=b):
                    for tt in range(4):
                        ins = PE.transpose(PS[:, b, tt * 128:(tt + 1) * 128], xs[s][:, tt, kc * 128:(kc + 1) * 128], ident)
                    return ins
                P.op('pe', f, reads=[('xs', s), 'ident'], writes=[('ps', b)])
                P.op('act', lambda kc=kc, tg=tg, b=b: ACT.activation(out=h1T[:, kc, tg * 512:(tg + 1) * 512], in_=PS[:, b, :],
                                                                     func=AF.Identity, bias=col(kc), scale=onep[:, kc:kc + 1]),
                     reads=[('ps', b), 'modcol', 'onep'], writes=[('h1T', kc, tg)])
        phase_end('xs0', 'xs1')

        STGW = 2048
        stg = [A.f32('stg%d' % i, STGW) for i in range(2)]
        stg_rr = [0]

        def load_stage(src_ap, kch, ncols):
            assert kch * ncols <= STGW
            s = stg_rr[0] % 2
            stg_rr[0] += 1
            view = stg[s][:, 0:kch * ncols].rearrange("p (k f) -> p k f", k=kch)
            dma(view, src_ap.rearrange("(k p) f -> p k f", p=128), ('stg', s), writes=[('stg', s)])
            return s, view

        def load_w_bf16(dst3, src_ap, kch, ncols, wkey, eng='pool'):
            kstep = max(1, STGW // ncols)
            for k0 in range(0, kch, kstep):
                k1 = min(kch, k0 + kstep)
                s, view = load_stage(src_ap[k0 * 128:k1 * 128, :], k1 - k0, ncols)
                if eng == 'pool':
                    P.op('pool', lambda view=view, k0=k0, k1=k1: G.tensor_copy(out=dst3[:, k0:k1, :], in_=view),
                         reads=[('stg', s)], writes=[wkey])
                else:
                    P.op('dve', lambda view=view, k0=k0, k1=k1: V.tensor_copy(out=dst3[:, k0:k1, :], in_=view),
                         reads=[('stg', s)], writes=[wkey])

        qfT = A.bf16('qfT', 8 * S).rearrange("p (h t) -> p h t", h=8)
        kfT = A.bf16('kfT', 8 * S).rearrange("p (h t) -> p h t", h=8)
        vB_flat = A.bf16('vB', NT * 8 * 65 + 8)
        vB = vB_flat[:, 0:NT * 8 * 65].rearrange("p (t h e) -> p t h e", t=NT, h=8)
        P.op('pool', lambda: G.memset(vB_flat, 1.0), writes=['vB'])
        P.op('pool', lambda: G.memset(qfT.rearrange("p h t -> p (h t)"), 0.0), writes=['qfT_init'])

        wcq = A.bf16('wcq', 8 * 384).rearrange("p (k f) -> p k f", k=8)
        wckv = A.bf16('wckv', 8 * 256).rearrange("p (k f) -> p k f", k=8)
        wkr = A.bf16('wkr', 8 * 128).rearrange("p (k f) -> p k f", k=8)
        wkrr = A.bf16('wkrr', 8 * 128).rearrange("p (k f) -> p k f", k=8)
        Wqf = A.bf16('Wqf', 3 * 8 * 128).rearrange("p (k h f) -> p k h f", k=3, h=8)
        Wqr = A.bf16('Wqr', 3 * 8 * 128).rearrange("p (k h f) -> p k h f", k=3, h=8)
        Wkf = A.bf16('Wkf', 2 * 8 * 128).rearrange("p (k h f) -> p k h f", k=2, h=8)
        Wv = A.bf16('Wv', 2 * 512).rearrange("p (k f) -> p k f", k=2)
        rmsq = A.f32('rmsq', 3)
        rmskv = A.f32('rmskv', 2)
        freq = A.f32('freq', 1)
        dma(rmsq, rmsq_d, 'rmsq', writes=['rmsq'])
        dma(rmskv, rmskv_d, 'rmskv', writes=['rmskv'])
        dma(freq, freq_d, 'freq', writes=['freq'])

        load_w_bf16(wcq, win_d[:, 1536:1920], 8, 384, 'wcq')
        load_w_bf16(wckv, win_d[:, 1920:2176], 8, 256, 'wckv')
        P.op('pool', lambda: G.memset(wkr.rearrange("p k f -> p (k f)"), 0.0), writes=['wkr'])
        P.op('pool', lambda: G.memset(wkrr.rearrange("p k f -> p (k f)"), 0.0), writes=['wkrr'])
        s, view = load_stage(win_d[:, 2176:2208], 8, 32)
        P.op('dve', lambda view=view: V.tensor_copy(out=wkr[:, :, 64:96], in_=view), reads=[('stg', s), 'wkr'], writes=['wkr'])
        P.op('dve', lambda view=view: V.tensor_scalar(out=wkrr[:, :, 64:80], in0=view[:, :, 16:32], scalar1=-1.0, scalar2=None, op0=ALU.mult),
             reads=[('stg', s), 'wkrr'], writes=['wkrr'])
        P.op('dve', lambda view=view: V.tensor_copy(out=wkrr[:, :, 80:96], in_=view[:, :, 0:16]), reads=[('stg', s), 'wkrr'], writes=['wkrr'])
        P.op('pool', lambda: G.memset(Wqf.rearrange("p k h f -> p (k h f)"), 0.0), writes=['Wqf'])
        P.op('pool', lambda: G.memset(Wqr.rearrange("p k h f -> p (k h f)"), 0.0), writes=['Wqr'])
        for k in range(3):
            s, view = load_stage(wuq_d[k * 128:(k + 1) * 128, :], 1, 768)
            v4 = view[:, 0, :].rearrange("p (h f) -> p h f", h=8)
            P.op('dve', lambda k=k, v4=v4: V.tensor_scalar(out=Wqf[:, k, :, 0:96], in0=v4, scalar1=rmsq[:, k:k + 1], scalar2=None, op0=ALU.mult),
                 reads=[('stg', s), 'rmsq', 'Wqf'], writes=['Wqf'])
            P.op('dve', lambda k=k, v4=v4: V.tensor_scalar(out=Wqr[:, k, :, 64:80], in0=v4[:, :, 80:96], scalar1=rmsq[:, k:k + 1], scalar2=-1.0,
                                                           op0=ALU.mult, op1=ALU.mult),
                 reads=[('stg', s), 'rmsq', 'Wqr'], writes=['Wqr'])
            P.op('dve', lambda k=k, v4=v4: V.tensor_scalar(out=Wqr[:, k, :, 80:96], in0=v4[:, :, 64:80], scalar1=rmsq[:, k:k + 1], scalar2=None,
                                                           op0=ALU.mult),
                 reads=[('stg', s), 'rmsq', 'Wqr'], writes=['Wqr'])
        P.op('pool', lambda: G.memset(Wkf.rearrange("p k h f -> p (k h f)"), 0.0), writes=['Wkf'])
        for k in range(2):
            s, view = load_stage(wukv_d[k * 128:(k + 1) * 128, :], 1, 1024)
            v4 = view[:, 0, :].rearrange("p (h f) -> p h f", h=8)
            P.op('dve', lambda k=k, v4=v4: V.tensor_scalar(out=Wkf[:, k, :, 0:64], in0=v4[:, :, 0:64], scalar1=rmskv[:, k:k + 1], scalar2=None, op0=ALU.mult),
                 reads=[('stg', s), 'rmskv', 'Wkf'], writes=['Wkf'])
            P.op('dve', lambda k=k, v4=v4: V.tensor_scalar(out=Wv[:, k, :].rearrange("p (h f) -> p h f", h=8), in0=v4[:, :, 64:128],
                                                           scalar1=rmskv[:, k:k + 1], scalar2=None, op0=ALU.mult),
                 reads=[('stg', s), 'rmskv'], writes=['Wv'])

        posi = A.i32('posi', 512)
        ang = A.f32('ang', 512)
        tC = A.f32('tC', 512)
        tS = A.f32('tS', 512)
        tmpA = A.f32('tmpA', 512)
        tmpB = A.f32('tmpB', 512)
        tmpA2 = A.f32('tmpA2', 512)
        tmpB2 = A.f32('tmpB2', 512)
        cqT = A.bf16('cqT', 3 * 512).rearrange("p (k t) -> p k t", k=3)
        ckvT = A.bf16('ckvT', 2 * 512).rearrange("p (k t) -> p k t", k=2)
        sqt = [A.bf16('sqt%d' % i, 512) for i in range(3)]
        rq = A.f32('rq', 512)
        rkv = A.f32('rkv', 512)
        Cq = A.f32('Cq', 512)
        Sq = A.f32('Sq', 512)
        Ck = A.f32('Ck', 512)
        kper = A.bf16('kper', 512)
        rkc = A.f32('rkc', 8)
        P.op('pool', lambda: G.memset(Sq, 0.0), writes=['Sq'])
        P.op('pool', lambda: G.memset(Cq, 0.0), writes=['Cq'])
        P.op('pool', lambda: G.memset(Ck, 0.0), writes=['Ck'])

        for tg in range(NG):
            tsl = slice(tg * 512, (tg + 1) * 512)
            dma(posi, pos_d[0:1, tsl].to_broadcast([128, 512]), 'posi', writes=['posi'])
            P.op('dve', lambda: V.tensor_copy(out=ang, in_=posi), reads=['posi'], writes=['ang'])
            P.op('dve', lambda: V.tensor_scalar(out=ang, in0=ang, scalar1=freq[:, 0:1], scalar2=None, op0=ALU.mult),
                 reads=['ang', 'freq'], writes=['ang'])
            P.op('dve', lambda: V.tensor_scalar(out=posi, in0=ang, scalar1=1.0 / TWO_PI, scalar2=None, op0=ALU.mult),
                 reads=['ang', 'posi'], writes=['posi'])
            P.op('dve', lambda: V.tensor_copy(out=tmpB, in_=posi), reads=['posi'], writes=['tmpB'])
            P.op('dve', lambda: V.scalar_tensor_tensor(out=ang, in0=tmpB, scalar=-CW1, in1=ang, op0=ALU.mult, op1=ALU.add),
                 reads=['tmpB', 'ang'], writes=['ang'])
            P.op('dve', lambda: V.scalar_tensor_tensor(out=ang, in0=tmpB, scalar=-CW2, in1=ang, op0=ALU.mult, op1=ALU.add),
                 reads=['tmpB', 'ang'], writes=['ang'])
            for (tab, shift) in ((tS, 0.0), (tC, math.pi / 2)):
                P.op('dve', lambda tab=tab, shift=shift: V.tensor_scalar(out=tmpA, in0=ang, scalar1=shift, scalar2=None, op0=ALU.add),
                     reads=['ang'], writes=['tmpA'])
                P.op('dve', lambda: V.tensor_scalar(out=tmpB, in0=tmpA, scalar1=math.pi, scalar2=TWO_PI, op0=ALU.is_gt, op1=ALU.mult),
                     reads=['tmpA'], writes=['tmpB'])
                P.op('dve', lambda: V.tensor_tensor(out=tmpA, in0=tmpA, in1=tmpB, op=ALU.subtract), reads=['tmpA', 'tmpB'], writes=['tmpA'])
                P.op('dve', lambda: V.tensor_scalar(out=tmpB, in0=tmpA, scalar1=-math.pi, scalar2=TWO_PI, op0=ALU.is_lt, op1=ALU.mult),
                     reads=['tmpA'], writes=['tmpB'])
                P.op('dve', lambda: V.tensor_tensor(out=tmpA, in0=tmpA, in1=tmpB, op=ALU.add), reads=['tmpA', 'tmpB'], writes=['tmpA'])
                P.op('dve', lambda: V.tensor_scalar(out=tmpA, in0=tmpA, scalar1=PI_LO, scalar2=-PI_LO, op0=ALU.min, op1=ALU.max),
                     reads=['tmpA'], writes=['tmpA'])
                P.op('act', lambda tab=tab: ACT.activation(out=tab, in_=tmpA, func=AF.Sin), reads=['tmpA'], writes=[('tab', id(tab))])

            bq = nb()
            for m in range(3):
                b = nb()

                def f(m=m, b=b, tg=tg):
                    for kc in range(8):
                        ins = PE.matmul(PS[:, b, :], lhsT=wcq[:, kc, m * 128:(m + 1) * 128], rhs=h1T[:, kc, tg * 512:(tg + 1) * 512],
                                        start=(kc == 0), stop=(kc == 7))
                    return ins
                P.op('pe', f, reads=['wcq'] + [('h1T', kc, tg) for kc in range(8)], writes=[('ps', b)])
                P.op('act', lambda m=m, b=b: ACT.copy(out=cqT[:, m, :], in_=PS[:, b, :]), reads=[('ps', b)], writes=[('cqT', m)])
                P.op('act', lambda m=m, b=b: ACT.activation(out=sqt[m], in_=PS[:, b, :], func=AF.Square), reads=[('ps', b)], writes=[('sqt', m)])
                P.op('pe', lambda m=m, bq=bq: PE.matmul(PS[:, bq, :], lhsT=ones_bf, rhs=sqt[m], start=(m == 0), stop=(m == 2)),
                     reads=['ones_bf', ('sqt', m)], writes=[('ps', bq)])
            P.op('act', lambda bq=bq: ACT.activation(out=rq, in_=PS[:, bq, :], func=AF.Sqrt, bias=RMS_EPS, scale=1.0 / 384.0),
                 reads=[('ps', bq)], writes=['rq'])
            P.op('dve', lambda: V.reciprocal(out=rq, in_=rq), reads=['rq'], writes=['rq'])
            P.op('dve', lambda: V.tensor_copy(out=Cq[0:64, :], in_=rq[0:64, :]), reads=['rq'], writes=['Cq'])
            P.op('dve', lambda: V.tensor_tensor(out=Cq[64:96, :], in0=tC[64:96, :], in1=rq[64:96, :], op=ALU.mult),
                 reads=['rq', ('tab', id(tC)), 'Cq'], writes=['Cq'])
            P.op('dve', lambda: V.tensor_tensor(out=Sq[64:96, :], in0=tS[64:96, :], in1=rq[64:96, :], op=ALU.mult),
                 reads=['rq', ('tab', id(tS))], writes=['Sq'])
            for h in range(8):
                ba = nb()
                bb = nb()

                def f(h=h, ba=ba, bb=bb):
                    for m in range(3):
                        PE.matmul(PS[:, ba, :], lhsT=Wqf[:, m, h, :], rhs=cqT[:, m, :], start=(m == 0), stop=(m == 2))
                    for m in range(3):
                        ins = PE.matmul(PS[:, bb, :], lhsT=Wqr[:, m, h, :], rhs=cqT[:, m, :], start=(m == 0), stop=(m == 2))
                    return ins
                P.op('pe', f, reads=['Wqf', 'Wqr'] + [('cqT', m) for m in range(3)], writes=[('ps', ba), ('ps', bb)])
                tA, tB, kA, kB = (tmpA, tmpB, 'tmpA', 'tmpB') if h % 2 == 0 else (tmpA2, tmpB2, 'tmpA2', 'tmpB2')
                P.op('dve', lambda ba=ba, tA=tA: V.tensor_tensor(out=tA, in0=PS[:, ba, :], in1=Cq, op=ALU.mult), reads=[('ps', ba), 'Cq'], writes=[kA])
                P.op('dve', lambda bb=bb, tB=tB: V.tensor_tensor(out=tB, in0=PS[:, bb, :], in1=Sq, op=ALU.mult), reads=[('ps', bb), 'Sq'], writes=[kB])
                P.op('pool', lambda h=h, tsl=tsl, tA=tA, tB=tB: G.tensor_tensor(out=qfT[:, h, tsl], in0=tA, in1=tB, op=ALU.add),
                     reads=[kA, kB, 'qfT_init'], writes=[('qfT', h, tg)])

            bq = nb()
            bcol = nb()
            for m in range(2):
                b = nb()

                def f(m=m, b=b, tg=tg):
                    for kc in range(8):
                        ins = PE.matmul(PS[:, b, :], lhsT=wckv[:, kc, m * 128:(m + 1) * 128], rhs=h1T[:, kc, tg * 512:(tg + 1) * 512],
                                        start=(kc == 0), stop=(kc == 7))
                    return ins
                P.op('pe', f, reads=['wckv'] + [('h1T', kc, tg) for kc in range(8)], writes=[('ps', b)])
                P.op('act', lambda m=m, b=b: ACT.copy(out=ckvT[:, m, :], in_=PS[:, b, :]), reads=[('ps', b)], writes=[('ckvT', m)])
                P.op('act', lambda m=m, b=b: ACT.activation(out=sqt[m], in_=PS[:, b, :], func=AF.Square), reads=[('ps', b)], writes=[('sqt', m)])
                P.op('pe', lambda m=m, bq=bq: PE.matmul(PS[:, bq, :], lhsT=ones_bf, rhs=sqt[m], start=(m == 0), stop=(m == 1)),
                     reads=['ones_bf', ('sqt', m)], writes=[('ps', bq)])
            def f(bcol=bcol):
                first = True
                for tt in range(4):
                    for m in range(2):
                        ins = PE.matmul(PS[:, bcol, 2 * tt:2 * tt + 2], lhsT=sqt[m][:, tt * 128:(tt + 1) * 128], rhs=ones_bf[:, 0:2],
                                        start=first, stop=(tt == 3 and m == 1), skip_group_check=True)
                        first = False
                return ins
            P.op('pe', f, reads=['ones_bf', ('sqt', 0), ('sqt', 1)], writes=[('ps', bcol)])
            P.op('act', lambda bq=bq: ACT.activation(out=rkv, in_=PS[:, bq, :], func=AF.Sqrt, bias=RMS_EPS, scale=1.0 / 256.0),
                 reads=[('ps', bq)], writes=['rkv'])
            P.op('dve', lambda: V.reciprocal(out=rkv, in_=rkv), reads=['rkv'], writes=['rkv'])
            P.op('act', lambda bcol=bcol: ACT.activation(out=rkc, in_=PS[:, bcol, 0:8], func=AF.Sqrt, bias=RMS_EPS, scale=1.0 / 256.0),
                 reads=[('ps', bcol)], writes=['rkc'])
            P.op('dve', lambda: V.reciprocal(out=rkc, in_=rkc), reads=['rkc'], writes=['rkc'])
            P.op('dve', lambda: V.tensor_copy(out=Ck[0:64, :], in_=rkv[0:64, :]), reads=['rkv'], writes=['Ck'])
            ba = nb()
            bb = nb()

            def f(ba=ba, bb=bb, tg=tg):
                for kc in range(8):
                    PE.matmul(PS[:, ba, :], lhsT=wkr[:, kc, :], rhs=h1T[:, kc, tg * 512:(tg + 1) * 512], start=(kc == 0), stop=(kc == 7))
                for kc in range(8):
                    ins = PE.matmul(PS[:, bb, :], lhsT=wkrr[:, kc, :], rhs=h1T[:, kc, tg * 512:(tg + 1) * 512], start=(kc == 0), stop=(kc == 7))
                return ins
            P.op('pe', f, reads=['wkr', 'wkrr'] + [('h1T', kc, tg) for kc in range(8)], writes=[('ps', ba), ('ps', bb)])
            P.op('dve', lambda ba=ba: V.tensor_tensor(out=tmpA[64:96, :], in0=PS[64:96, ba, :], in1=tC[64:96, :], op=ALU.mult),
                 reads=[('ps', ba), ('tab', id(tC))], writes=['tmpA'])
            P.op('dve', lambda bb=bb: V.tensor_tensor(out=tmpB[64:96, :], in0=PS[64:96, bb, :], in1=tS[64:96, :], op=ALU.mult),
                 reads=[('ps', bb), ('tab', id(tS))], writes=['tmpB'])
            P.op('dve', lambda: V.tensor_tensor(out=kper[64:96, :], in0=tmpA[64:96, :], in1=tmpB[64:96, :], op=ALU.add),
                 reads=['tmpA', 'tmpB'], writes=['kper'])
            for h in range(8):
                ba = nb()

                def f(h=h, ba=ba):
                    for m in range(2):
                        ins = PE.matmul(PS[:, ba, :], lhsT=Wkf[:, m, h, :], rhs=ckvT[:, m, :], start=(m == 0), stop=(m == 1))
                    return ins
                P.op('pe', f, reads=['Wkf', ('ckvT', 0), ('ckvT', 1)], writes=[('ps', ba)])
                P.op('dve', lambda h=h, ba=ba, tsl=tsl: V.tensor_tensor(out=kfT[:, h, tsl], in0=PS[:, ba, :], in1=Ck, op=ALU.mult),
                     reads=[('ps', ba), 'Ck'], writes=[('kfT', h, tg)])
                P.op('pool', lambda h=h, tsl=tsl: G.tensor_copy(out=kfT[64:96, h, tsl], in_=kper[64:96, :]),
                     reads=['kper', ('kfT', h, tg)], writes=[('kfT', h, tg)])
            for tt in range(4):
                t = tg * 4 + tt
                ba = nb()

                def f(tt=tt, ba=ba):
                    for m in range(2):
                        ins = PE.matmul(PS[:, ba, :], lhsT=ckvT[:, m, tt * 128:(tt + 1) * 128], rhs=Wv[:, m, :], start=(m == 0), stop=(m == 1))
                    return ins
                P.op('pe', f, reads=['Wv', ('ckvT', 0), ('ckvT', 1)], writes=[('ps', ba)])
                P.op('dve', lambda t=t, tt=tt, ba=ba: V.tensor_scalar(out=vB[:, t, :, 0:64], in0=PS[:, ba, :].rearrange("p (h e) -> p h e", h=8),
                                                                    scalar1=rkc[:, 2 * tt:2 * tt + 1], scalar2=None, op0=ALU.mult),
                     reads=[('ps', ba), 'rkc', 'vB'], writes=[('vB', t)])

        phase_end('wcq', 'wckv', 'wkr', 'wkrr', 'Wqf', 'Wqr', 'Wkf', 'Wv', 'posi', 'ang', 'tC', 'tS', 'tmpA', 'tmpB', 'tmpA2', 'tmpB2',
                  'cqT', 'ckvT', 'sqt0', 'sqt1', 'sqt2', 'rq', 'rkv', 'Cq', 'Sq', 'Ck', 'kper', 'rkc')

        yT_b = A.bf16('yT_b', 4 * S).rearrange("p (k t) -> p k t", k=4)
        wq = A.bf16('wq', 8 * 512).rearrange("p (k f) -> p k f", k=8)
        wk = A.bf16('wk', 8 * 512).rearrange("p (k f) -> p k f", k=8)
        wv = A.bf16('wv', 8 * 512).rearrange("p (k f) -> p k f", k=8)
        load_w_bf16(wq, win_d[:, 0:512], 8, 512, 'wq')
        load_w_bf16(wk, win_d[:, 512:1024], 8, 512, 'wk')
        load_w_bf16(wv, win_d[:, 1024:1536], 8, 512, 'wv')
        ytok = A.bf16('ytok', NT * 512).rearrange("p (t f) -> p t f", t=NT)
        pT = [A.bf16('pT%d' % i, 512) for i in range(4)]
        rc = A.f32('rc', 4)
        SCL_B = 96.0 ** -0.5
        LOOK = 2
        its = []
        for h in range(8):
            for g in range(NG):
                nkt = 4 * g + 4
                for kt in range(nkt):
                    its.append(dict(h=h, g=g, kt=kt, first=(kt == 0), last=(kt == nkt - 1), bo=6 + (h * NG + g) % 2))

        def mla_score(i):
            it = its[i]
            h, g, kt = it['h'], it['g'], it['kt']
            j0 = max(0, kt - 4 * g)
            bs = nb(0, 6)
            sl = i % 4
            it['sl'] = sl
            it['j0'] = j0
            qs = slice(g * 512 + j0 * 128, (g + 1) * 512)
            P.op('pe', lambda: PE.matmul(PS[:, bs, j0 * 128:512], lhsT=kfT[:, h, kt * 128:(kt + 1) * 128], rhs=qfT[:, h, qs], start=True, stop=True),
                 reads=[('kfT', h, kt // 4), ('qfT', h, g)], writes=[('ps', bs)])
            P.op('act', lambda: ACT.activation(out=pT[sl][:, j0 * 128:512], in_=PS[:, bs, j0 * 128:512], func=AF.Exp, scale=SCL_B),
                 reads=[('ps', bs)], writes=[('pT', sl)])
            if kt >= 4 * g:
                P.op('dve', lambda: V.memset(pT[sl][64:128, j0 * 128:j0 * 128 + 64], 0.0), reads=[('pT', sl)], writes=[('pT', sl)])

        def mla_pv(i):
            it = its[i]
            h, g, kt, sl, j0, bo = it['h'], it['g'], it['kt'], it['sl'], it['j0'], it['bo']
            PSO = PS[:, bo, 0:260].rearrange("p (j e) -> p j e", e=65)

            def f():
                fp = it['first']
                for j in range(j0, 4):
                    ins = PE.matmul(PSO[:, j, :], lhsT=pT[sl][:, j * 128:(j + 1) * 128], rhs=vB[:, kt, h, :],
                                    start=fp, stop=(it['last'] and j == 3), skip_group_check=True)
                    fp = False
                return ins
            P.op('pe', f, reads=[('pT', sl), ('vB', kt)], writes=[('ps', bo)])
            if it['last']:
                P.op('dve', lambda: V.reciprocal(out=rc, in_=PSO[:, :, 64]), reads=[('ps', bo)], writes=['rc'])
                for j in range(4):
                    t = 4 * g + j
                    P.op('dve', lambda j=j, t=t: V.tensor_scalar(out=ytok[:, t, h * 64:(h + 1) * 64], in0=PSO[:, j, 0:64],
                                                                 scalar1=rc[:, j:j + 1], scalar2=None, op0=ALU.mult),
                         reads=[('ps', bo), 'rc'], writes=[('ytok', t)])

        for i in range(len(its) + LOOK):
            if i < len(its):
                mla_score(i)
            if i - LOOK >= 0:
                mla_pv(i - LOOK)
        def ytok_to_T(yT):
            for c in range(4):
                for g in range(NG):
                    b = nb()

                    def f(c=c, g=g, b=b):
                        for j in range(4):
                            ins = PE.matmul(PS[:, b, j * 128:(j + 1) * 128], lhsT=ytok[:, 4 * g + j, c * 128:(c + 1) * 128], rhs=identb,
                                            start=True, stop=True)
                        return ins
                    P.op('pe', f, reads=['identb'] + [('ytok', 4 * g + j) for j in range(4)], writes=[('ps', b)])
                    P.op('act', lambda c=c, g=g, b=b: ACT.copy(out=yT[:, c, g * 512:(g + 1) * 512], in_=PS[:, b, :]),
                         reads=[('ps', b)], writes=[('yT', id(yT), c, g)])
        ytok_to_T(yT_b)
        phase_end('qfT', 'kfT', 'vB', 'pT0', 'pT1', 'pT2', 'pT3', 'rmsq', 'rmskv', 'freq')

        qT = A.bf16('qT', 4 * S).rearrange("p (k t) -> p k t", k=4)
        kT = A.bf16('kT', 4 * S).rearrange("p (k t) -> p k t", k=4)
        vA_flat = A.bf16('vA', NT * 8 * 65 + 8)
        vA = vA_flat[:, 0:NT * 8 * 65].rearrange("p (t h e) -> p t h e", t=NT, h=8)
        P.op('pool', lambda: G.memset(vA_flat, 1.0), writes=['vA'])
        maskT = A.f32('maskT', 640)
        expB = A.bf16('expB', 5120).rearrange("p (h d q) -> p h d q", h=8, d=5)
        bTt = [A.f32('bTt%d' % i, 640) for i in range(2)]
        dma(maskT, maskT_d, 'maskT', writes=['maskT'])
        for h in range(8):
            s2 = h % 2
            dma(bTt[s2], biasT_d[:, h * 640:(h + 1) * 640], ('bTt', s2), writes=[('bTt', s2)])
            P.op('act', lambda s2=s2: ACT.activation(out=bTt[s2], in_=bTt[s2], func=AF.Exp), reads=[('bTt', s2)], writes=[('bTt', s2)])
            P.op('dve', lambda h=h, s2=s2: V.tensor_tensor(out=expB[:, h, :, :].rearrange("p d q -> p (d q)"), in0=bTt[s2], in1=maskT, op=ALU.mult),
                 reads=[('bTt', s2), 'maskT'], writes=['expB'])
        for (wt, wkey, dst, dkey) in ((wq, 'wq', qT, 'qT'), (wk, 'wk', kT, 'kT')):
            for c in range(4):
                for g in range(NG):
                    b = nb()

                    def f(wt=wt, c=c, g=g, b=b):
                        for kc in range(8):
                            ins = PE.matmul(PS[:, b, :], lhsT=wt[:, kc, c * 128:(c + 1) * 128], rhs=h1T[:, kc, g * 512:(g + 1) * 512],
                                            start=(kc == 0), stop=(kc == 7))
                        return ins
                    P.op('pe', f, reads=[wkey] + [('h1T', kc, g) for kc in range(8)], writes=[('ps', b)])
                    P.op('act', lambda dst=dst, c=c, g=g, b=b: ACT.copy(out=dst[:, c, g * 512:(g + 1) * 512], in_=PS[:, b, :]),
                         reads=[('ps', b)], writes=[(dkey, c, g)])
        for t in range(NT):
            b = nb()

            def f(t=t, b=b):
                for kc in range(8):
                    ins = PE.matmul(PS[:, b, :], lhsT=h1T[:, kc, t * 128:(t + 1) * 128], rhs=wv[:, kc, :], start=(kc == 0), stop=(kc == 7))
                return ins
            P.op('pe', f, reads=['wv'] + [('h1T', kc, t // 4) for kc in range(8)], writes=[('ps', b)])
            P.op('dve', lambda t=t, b=b: V.tensor_copy(out=vA[:, t, :, 0:64], in_=PS[:, b, :].rearrange("p (h e) -> p h e", h=8)),
                 reads=[('ps', b), 'vA'], writes=[('vA', t)])
        phase_end('wq', 'wk', 'wv', 'bTt0', 'bTt1', 'maskT')
        yT_a = A.bf16('yT_a', 4 * S).rearrange("p (k t) -> p k t", k=4)
        et = [A.f32('et%d' % i, 512) for i in range(3)]
        wga = A.bf16('wga', 8 * D).rearrange("p (k f) -> p k f", k=8)
        wgb = A.bf16('wgb', 8 * D).rearrange("p (k f) -> p k f", k=8)
        load_w_bf16(wga, win_d[:, 2208:3232], 8, D, 'wga')
        load_w_bf16(wgb, win_d[:, 3232:4256], 8, D, 'wgb')
        pTa = [A.bf16('pTa%d' % i, 512) for i in range(4)]
        itsA = []
        for h in range(8):
            for g in range(NG):
                ds = [d for d in range(5) if any(4 * g + j - d >= 0 for j in range(4))]
                for d in ds:
                    itsA.append(dict(h=h, g=g, d=d, first=(d == ds[0]), last=(d == ds[-1]), bo=6 + (h * NG + g) % 2))

        def a_score(i):
            it = itsA[i]
            h, g, d = it['h'], it['g'], it['d']
            c = h // 2
            po = (h % 2) * 64
            js = [j for j in range(4) if 4 * g + j - d >= 0]
            j0 = js[0]
            nj = 4 - j0
            bs = nb(0, 6)
            es = i % 3
            sl = i % 4
            it['js'] = js
            it['sl'] = sl

            def f():
                for j in js:
                    T = 4 * g + j
                    ins = PE.matmul(PS[:, bs, j * 128:(j + 1) * 128], lhsT=kT[po:po + 64, c, (T - d) * 128:(T - d + 1) * 128],
                                    rhs=qT[po:po + 64, c, T * 128:(T + 1) * 128], start=True, stop=True)
                return ins
            kgs = sorted(set((4 * g + j - d) // 4 for j in js))
            P.op('pe', f, reads=[('kT', c, kg) for kg in kgs] + [('qT', c, g)], writes=[('ps', bs)])
            P.op('act', lambda: ACT.activation(out=et[es][:, j0 * 128:512], in_=PS[:, bs, j0 * 128:512], func=AF.Exp, scale=0.125),
                 reads=[('ps', bs)], writes=[('et', es)])
            P.op('dve', lambda: V.tensor_tensor(
                out=pTa[sl][:, j0 * 128:512].rearrange("p (j q) -> p j q", j=nj),
                in0=et[es][:, j0 * 128:512].rearrange("p (j q) -> p j q", j=nj),
                in1=expB[:, h, d, :].unsqueeze(1).to_broadcast([128, nj, 128]), op=ALU.mult),
                reads=[('et', es), 'expB'], writes=[('pTa', sl)])

        def a_pv(i):
            it = itsA[i]
            h, g, d, js, sl, bo = it['h'], it['g'], it['d'], it['js'], it['sl'], it['bo']
            PSO = PS[:, bo, 0:260].rearrange("p (j e) -> p j e", e=65)

            def f():
                fp = it['first']
                for j in js:
                    T = 4 * g + j
                    ins = PE.matmul(PSO[:, j, :], lhsT=pTa[sl][:, j * 128:(j + 1) * 128], rhs=vA[:, T - d, h, :],
                                    start=fp, stop=(it['last'] and j == js[-1]), skip_group_check=True)
                    fp = False
                return ins
            P.op('pe', f, reads=[('pTa', sl)] + [('vA', 4 * g + j - d) for j in js], writes=[('ps', bo)])
            if it['last']:
                P.op('dve', lambda: V.reciprocal(out=rc, in_=PSO[:, :, 64]), reads=[('ps', bo)], writes=['rc'])
                for j in range(4):
                    t = 4 * g + j
                    P.op('dve', lambda j=j, t=t: V.tensor_scalar(out=ytok[:, t, h * 64:(h + 1) * 64], in0=PSO[:, j, 0:64],
                                                                 scalar1=rc[:, j:j + 1], scalar2=None, op0=ALU.mult),
                         reads=[('ps', bo), 'rc'], writes=[('ytok', t)])

        for i in range(len(itsA) + LOOK):
            if i < len(itsA):
                a_score(i)
            if i - LOOK >= 0:
                a_pv(i - LOOK)
        ytok_to_T(yT_a)
        phase_end('qT', 'kT', 'vA', 'expB', 'et0', 'et1', 'et2', 'pTa0', 'pTa1', 'pTa2', 'pTa3', 'ytok', 'rc')

        mT = A.bf16('mT', 8 * S).rearrange("p (k t) -> p k t", k=8)
        wba = A.bf16('wba', 4 * D).rearrange("p (k f) -> p k f", k=4)
        wbb = A.bf16('wbb', 4 * D).rearrange("p (k f) -> p k f", k=4)
        load_w_bf16(wba, wba_d, 4, D, 'wba')
        load_w_bf16(wbb, wbb_d, 4, D, 'wbb')
        sga = [A.f32('sga%d' % i, 512) for i in range(2)]
        sgb = [A.f32('sgb%d' % i, 512) for i in range(2)]
        it = 0
        for fc in range(8):
            for g in range(NG):
                s2 = it % 2
                bset = (it % 2) * 4
                it += 1
                b_a, b_b, b_ga, b_gb = bset, bset + 1, bset + 2, bset + 3
                fsl = slice(fc * 128, (fc + 1) * 128)
                gsl = slice(g * 512, (g + 1) * 512)

                def f(fsl=fsl, gsl=gsl, b_ga=b_ga, b_gb=b_gb, b_a=b_a, b_b=b_b):
                    for kc in range(8):
                        PE.matmul(PS[:, b_ga, :], lhsT=wga[:, kc, fsl], rhs=h1T[:, kc, gsl], start=(kc == 0), stop=(kc == 7))
                    for kc in range(8):
                        PE.matmul(PS[:, b_gb, :], lhsT=wgb[:, kc, fsl], rhs=h1T[:, kc, gsl], start=(kc == 0), stop=(kc == 7))
                    for m in range(4):
                        PE.matmul(PS[:, b_a, :], lhsT=wba[:, m, fsl], rhs=yT_a[:, m, gsl], start=(m == 0), stop=(m == 3))
                    for m in range(4):
                        ins = PE.matmul(PS[:, b_b, :], lhsT=wbb[:, m, fsl], rhs=yT_b[:, m, gsl], start=(m == 0), stop=(m == 3))
                    return ins
                P.op('pe', f, reads=['wga', 'wgb', 'wba', 'wbb'] + [('h1T', kc, g) for kc in range(8)]
                     + [('yT', id(yT_a), m, g) for m in range(4)] + [('yT', id(yT_b), m, g) for m in range(4)],
                     writes=[('ps', b_a), ('ps', b_b), ('ps', b_ga), ('ps', b_gb)])
                P.op('act', lambda s2=s2, b_ga=b_ga: ACT.activation(out=sga[s2], in_=PS[:, b_ga, :], func=AF.Sigmoid), reads=[('ps', b_ga)], writes=[('sga', s2)])
                P.op('act', lambda s2=s2, b_gb=b_gb: ACT.activation(out=sgb[s2], in_=PS[:, b_gb, :], func=AF.Sigmoid), reads=[('ps', b_gb)], writes=[('sgb', s2)])
                P.op('dve', lambda s2=s2, b_a=b_a: V.tensor_tensor(out=sga[s2], in0=PS[:, b_a, :], in1=sga[s2], op=ALU.mult),
                     reads=[('ps', b_a), ('sga', s2)], writes=[('sga', s2)])
                P.op('dve', lambda s2=s2, b_b=b_b: V.tensor_tensor(out=sgb[s2], in0=PS[:, b_b, :], in1=sgb[s2], op=ALU.mult),
                     reads=[('ps', b_b), ('sgb', s2)], writes=[('sgb', s2)])
                P.op('pool', lambda s2=s2, fc=fc, gsl=gsl: G.tensor_tensor(out=mT[:, fc, gsl], in0=sga[s2], in1=sgb[s2], op=ALU.add),
                     reads=[('sga', s2), ('sgb', s2)], writes=[('mT', fc, g)])
        phase_end('wba', 'wbb', 'wga', 'wgb', 'sga0', 'sga1', 'sgb0', 'sgb1', 'h1T', 'yT_a', 'yT_b')

        Z = A.f32('Z', NT * D, top=True).rearrange("p (t f) -> p t f", t=NT)
        wout = A.bf16('wout', 8 * D).rearrange("p (k f) -> p k f", k=8)
        for k0 in range(0, 8, 2):
            s_, view = load_stage(wout_d[k0 * 128:(k0 + 2) * 128, :], 2, D)
            P.op('pool', lambda view=view, k0=k0: G.tensor_tensor(out=wout[:, k0:k0 + 2, :], in0=view,
                                                                  in1=g1bc.unsqueeze(1).to_broadcast([128, 2, D]), op=ALU.mult),
                 reads=[('stg', s_), ('gbc', id(g1bc), 0), ('gbc', id(g1bc), 1)], writes=['wout'])
        lng = A.f32('lng', D)
        lnb = A.f32('lnb', D)
        dma(lng, ln_d[0:1, :].to_broadcast([128, D]), 'lng', writes=['lng'])
        dma(lnb, ln_d[1:2, :].to_broadcast([128, D]), 'lnb', writes=['lnb'])
        xr = [A.f32('xr%d' % i, D) for i in range(2)]
        ztmp = [A.f32('ztmp%d' % i, D) for i in range(2)]
        stats = A.f32('stats', 12)
        mv = A.f32('mv', 2)
        rstd = A.f32('rstd', 1)

        def layer_norm_tile(src, dst, gam, bet, skey, dkey, gkeys):
            P.op('dve', lambda: V.bn_stats(out=stats[:, 0:6], in_=src[:, 0:512]), reads=[skey], writes=['stats'])
            P.op('dve', lambda: V.bn_stats(out=stats[:, 6:12], in_=src[:, 512:1024]), reads=[skey, 'stats'], writes=['stats'])
            P.op('dve', lambda: V.bn_aggr(out=mv, in_=stats), reads=['stats'], writes=['mv'])
            P.op('act', lambda: ACT.activation(out=rstd, in_=mv[:, 1:2], func=AF.Sqrt, bias=LN_EPS, scale=1.0), reads=['mv'], writes=['rstd'])
            P.op('dve', lambda: V.reciprocal(out=rstd, in_=rstd), reads=['rstd'], writes=['rstd'])
            P.op('dve', lambda: V.tensor_scalar(out=src, in0=src, scalar1=mv[:, 0:1], scalar2=rstd[:, 0:1], op0=ALU.subtract, op1=ALU.mult),
                 reads=[skey, 'mv', 'rstd'], writes=[skey])
            P.op('pool', lambda: G.tensor_tensor(out=src, in0=src, in1=gam, op=ALU.mult), reads=[skey] + gkeys, writes=[skey])
            P.op('pool', lambda: G.tensor_tensor(out=dst, in0=src, in1=bet, op=ALU.add), reads=[skey] + gkeys, writes=[dkey])

        for t in range(NT):
            s2 = t % 2
            dma(xr[s2], x_d[t * 128:(t + 1) * 128, :], ('xr', s2), writes=[('xr', s2)])
            for hh in range(2):
                b = nb()

                def f(t=t, hh=hh, b=b):
                    for kc in range(8):
                        ins = PE.matmul(PS[:, b, :], lhsT=mT[:, kc, t * 128:(t + 1) * 128], rhs=wout[:, kc, hh * 512:(hh + 1) * 512],
                                        start=(kc == 0), stop=(kc == 7))
                    return ins
                P.op('pe', f, reads=['wout'] + [('mT', kc, t // 4) for kc in range(8)], writes=[('ps', b)])
                P.op('dve', lambda s2=s2, hh=hh, b=b: V.scalar_tensor_tensor(out=ztmp[s2][:, hh * 512:(hh + 1) * 512], in0=xr[s2][:, hh * 512:(hh + 1) * 512],
                                                                             scalar=ALPHA, in1=PS[:, b, :], op0=ALU.mult, op1=ALU.add),
                     reads=[('ps', b), ('xr', s2), ('ztmp', s2)], writes=[('ztmp', s2)])
            layer_norm_tile(ztmp[s2], Z[:, t, :], lng, lnb, ('ztmp', s2), ('Z', t), ['lng', 'lnb'])

        def finish_with_Z():
            for t in range(NT):
                dma(out_d[t * 128:(t + 1) * 128, :], Z[:, t, :], ('ost', t % 2), reads=[('Z', t)], writes=[('out', t)])
            P.op('sp', lambda: nc.sync.nop(), reads=[('out', t) for t in range(NT)])
            P.emit(st)
            return nc
        if stage == "x1":
            return finish_with_Z()
        phase_end('mT', 'wout', 'xr0', 'xr1', 'g1bc', 'lng', 'lnb', 'stg0', 'stg1', 'ztmp0', 'ztmp1')

        h2T = A.bf16('h2T', 8 * S, top=True).rearrange("p (k t) -> p k t", k=8)
        h2f = A.f32('h2f', 8 * 512).rearrange("p (k t) -> p k t", k=8)
        wr = A.f32('wr', 8 * NE).rearrange("p (k e) -> p k e", k=8)
        dma(wr, wr_d.rearrange("(k p) e -> p k e", p=128), 'wr', writes=['wr'])
        brbc = A.f32('brbc', NE)
        dma(brbc, br_d[0:1, :].to_broadcast([128, NE]), 'brbc', writes=['brbc'])
        RW = A.f32('RW', NT * NE, top=True).rearrange("p (t e) -> p t e", t=NT)
        RWT = A.f32('RWT', 128)
        bdg4 = A.f32('bdg4', 4 * D).rearrange("p (a f) -> p a f", a=4)
        P.op('pool', lambda: G.memset(bdg4.rearrange("p a f -> p (a f)"), 0.0), writes=['bdg4'])
        for a in range(4):
            dma(bdg4[a * NE:(a + 1) * NE, a, :], bdown_d, ('bdg4', a), reads=['bdg4'], writes=[('bdg4d', a)])
        P.op('dve', lambda: V.tensor_tensor(out=bdg4, in0=bdg4, in1=g2bc.unsqueeze(1).to_broadcast([128, 4, D]), op=ALU.mult),
             reads=['bdg4', ('gbc', id(g2bc), 0), ('gbc', id(g2bc), 1)] + [('bdg4d', a) for a in range(4)], writes=['bdg4f'])
        bg17 = A.f32('bg17', NE * 8, top=True)
        bgc = A.f32('bgc', NE * 8, top=True)
        bu1 = A.f32('bu1', NE * 8, top=True)
        dma(bgc, bgcol_d, 'bgc', writes=['bgc'])
        dma(bu1, bucol_d, 'bu1', writes=['bu1'])
        P.op('dve', lambda: V.tensor_scalar(out=bg17, in0=bgc, scalar1=1.702, scalar2=None, op0=ALU.mult), reads=['bgc'], writes=['bg17'])
        P.op('dve', lambda: V.tensor_scalar(out=bu1, in0=bu1, scalar1=1.0, scalar2=None, op0=ALU.add), reads=['bu1'], writes=['bu1'])
        Lg = A.f32('Lg', NE)
        m8 = A.f32('m8', 8)
        nmx = A.f32('nmx', 1)
        ex = A.f32('ex', NE)
        msk = A.f32('msk', NE)
        ssum = A.f32('ssum', 1)
        if stage == "p5a":
            return finish_with_Z()
        if stage == "full":
            NSL = 5
            Wsl = [A.bf16('Wsl%d' % i, 8 * D).rearrange("p (k f) -> p k f", k=8) for i in range(3)] + [None, None]
            ms = [A.f32('ms%d' % i, 512).rearrange("p (k f) -> p k f", k=1) for i in range(2)]
            chunk_rr = [0]
            wsrc = (wg_d, wu_d, wd_d)

            def load_matrix(mi):
                e, which = divmod(mi, 3)
                slot = mi % NSL
                for q in range(16):
                    s = chunk_rr[0] % 2
                    chunk_rr[0] += 1
                    kq, hq = q // 2, q % 2
                    dma(ms[s], wsrc[which][e, kq * 128:(kq + 1) * 128, hq * 512:(hq + 1) * 512].rearrange("(k p) f -> p k f", p=128), ('ms', s), writes=[('ms', s)])
                    if which == 2:
                        P.op('pool', lambda s=s, slot=slot, kq=kq, hq=hq: G.tensor_tensor(out=Wsl[slot][:, kq:kq + 1, hq * 512:(hq + 1) * 512], in0=ms[s],
                                                                                          in1=g2bc[:, hq * 512:(hq + 1) * 512].unsqueeze(1), op=ALU.mult),
                             reads=[('ms', s), ('gbc', id(g2bc), 0), ('gbc', id(g2bc), 1)], writes=[('W', slot)])
                    else:
                        P.op('pool', lambda s=s, slot=slot, kq=kq, hq=hq: G.tensor_copy(out=Wsl[slot][:, kq:kq + 1, hq * 512:(hq + 1) * 512], in_=ms[s]),
                             reads=[('ms', s)], writes=[('W', slot)])

            n_mat = 3 * n_exp
            uses_left = [NG * 8] * n_mat
            next_mat = [0]
            load_limit = [3]

            def pump_loads():
                while next_mat[0] < min(n_mat, load_limit[0]) and (next_mat[0] < NSL or uses_left[next_mat[0] - NSL] == 0):
                    load_matrix(next_mat[0])
                    next_mat[0] += 1

            pump_loads()

        for g in range(NG):
            gsl = slice(g * 512, (g + 1) * 512)
            for kc in range(8):
                b = nb()

                def f(g=g, kc=kc, b=b):
                    for tt in range(4):
                        ins = PE.transpose(PS[:, b, tt * 128:(tt + 1) * 128], Z[:, 4 * g + tt, kc * 128:(kc + 1) * 128], ident)
                    return ins
                if stage == "p5b" and g == P5G and kc > P5K:
                    continue
                if 'p' in P5M or not (stage == "p5b" and g == P5G):
                    P.op('pe', f, reads=['ident'] + [('Z', 4 * g + tt) for tt in range(4)], writes=[('ps', b)])
                if not ('a' in P5M or not (stage == "p5b" and g == P5G)):
                    continue
                P.op('act', lambda kc=kc, gsl=gsl, b=b: ACT.activation(out=h2T[:, kc, gsl], in_=PS[:, b, :], func=AF.Identity,
                                                                       bias=col(24 + kc), scale=onep[:, 8 + kc:9 + kc]),
                     reads=[('ps', b), 'modcol', 'onep'], writes=[('h2T', kc, g)])
                if not ('d' in P5M or not (stage == "p5b" and g == P5G)):
                    continue
                P.op('dve', lambda kc=kc, b=b: V.tensor_scalar(out=h2f[:, kc, :], in0=PS[:, b, :], scalar1=onep[:, 8 + kc:9 + kc], scalar2=col(24 + kc),
                                                               op0=ALU.mult, op1=ALU.add),
                     reads=[('ps', b), 'modcol', 'onep'], writes=[('h2f', kc)])
            if stage == "p5b" and g == P5G:
                return finish_with_Z()
            for tt in range(4):
                t = 4 * g + tt
                P.op('act', lambda t=t: ACT.mul(out=Z[:, t, :], in_=Z[:, t, :], mul=ALPHA), reads=[('Z', t)], writes=[('Z', t)])
                b = nb()

                def f(tt=tt, b=b):
                    for kc in range(8):
                        ins = PE.matmul(PS[:, b, 0:NE], lhsT=h2f[:, kc, tt * 128:(tt + 1) * 128], rhs=wr[:, kc, :], start=(kc == 0), stop=(kc == 7))
                    return ins
                P.op('pe', f, reads=['wr'] + [('h2f', kc) for kc in range(8)], writes=[('ps', b)])
                P.op('dve', lambda b=b: V.tensor_tensor(out=Lg, in0=PS[:, b, 0:NE], in1=brbc, op=ALU.add), reads=[('ps', b), 'brbc'], writes=['Lg'])
                P.op('dve', lambda: V.max(out=m8, in_=Lg), reads=['Lg'], writes=['m8'])
                P.op('dve', lambda: V.tensor_scalar(out=nmx, in0=m8[:, 0:1], scalar1=-1.0, scalar2=None, op0=ALU.mult), reads=['m8'], writes=['nmx'])
                P.op('act', lambda: ACT.activation(out=ex, in_=Lg, func=AF.Exp, bias=nmx[:, 0:1], scale=1.0), reads=['Lg', 'nmx'], writes=['ex'])
                P.op('dve', lambda: V.tensor_scalar(out=msk, in0=Lg, scalar1=m8[:, 3:4], scalar2=None, op0=ALU.is_ge), reads=['Lg', 'm8'], writes=['msk'])
                P.op('dve', lambda: V.tensor_tensor(out=ex, in0=ex, in1=msk, op=ALU.mult), reads=['ex', 'msk'], writes=['ex'])
                P.op('dve', lambda: V.reduce_sum(out=ssum, in_=ex, axis=mybir.AxisListType.X), reads=['ex'], writes=['ssum'])
                P.op('dve', lambda: V.reciprocal(out=ssum, in_=ssum), reads=['ssum'], writes=['ssum'])
                P.op('dve', lambda t=t: V.tensor_scalar(out=RW[:, t, :], in0=ex, scalar1=ssum[:, 0:1], scalar2=None, op0=ALU.mult),
                     reads=['ex', 'ssum'], writes=[('RW', t)])
            if stage == "p5c" and g == P5G:
                return finish_with_Z()
            b2 = nb()
            P.op('pe', lambda g=g, b2=b2: PE.transpose(PS[:, b2, 0:128], RW[:, 4 * g:4 * g + 4, :].rearrange("p t e -> p (t e)"), ident),
                 reads=[('RW', 4 * g + tt) for tt in range(4)] + ['ident'], writes=[('ps', b2)])
            P.op('act', lambda b2=b2: ACT.copy(out=RWT, in_=PS[:, b2, 0:128]), reads=[('ps', b2)], writes=['RWT'])
            if stage == "p5d" and g == P5G:
                return finish_with_Z()
            for tt in range(4):
                t = 4 * g + tt
                for hh in range(2):
                    b3 = nb()
                    P.op('pe', lambda tt=tt, hh=hh, b3=b3: PE.matmul(PS[:, b3, :], lhsT=RWT, rhs=bdg4[:, tt, hh * 512:(hh + 1) * 512],
                                                                    start=True, stop=True),
                         reads=['RWT', 'bdg4f'], writes=[('ps', b3)])
                    P.op('dve', lambda t=t, hh=hh, b3=b3: V.tensor_tensor(out=Z[:, t, hh * 512:(hh + 1) * 512], in0=PS[:, b3, :],
                                                                         in1=Z[:, t, hh * 512:(hh + 1) * 512], op=ALU.add),
                         reads=[('ps', b3), ('Z', t)], writes=[('Z', t)])
            if stage == "p5e" and g == P5G:
                return finish_with_Z()
        if stage == "p5":
            return finish_with_Z()
        phase_end('h2f', 'wr', 'brbc', 'RWT', 'bdg4', 'Lg', 'm8', 'nmx', 'ex', 'msk', 'ssum')

        for i in (3, 4):
            Wsl[i] = A.bf16('Wsl%d' % i, 8 * D).rearrange("p (k f) -> p k f", k=8)
        hTb = [A.bf16('hT%d' % i, 8 * 512).rearrange("p (k t) -> p k t", k=8) for i in range(2)]
        Sg = [A.bf16('Sg%d' % i, 512) for i in range(3)]
        Gs = [A.bf16('Gs%d' % i, 512) for i in range(3)]
        Us = [A.bf16('Us%d' % i, 512) for i in range(3)]
        load_limit[0] = 10 ** 9
        pump_loads()
        units = [(e, g) for e in range(n_exp) for g in range(NG)]
        a_cnt = [0]
        y_cnt = [0]

        def A_step(u, fc):
            e, g = units[u]
            sg_, su_ = (3 * e) % NSL, (3 * e + 1) % NSL
            Wg_, Wu_ = Wsl[sg_], Wsl[su_]
            hT = hTb[u % 2]
            gsl = slice(g * 512, (g + 1) * 512)
            s3 = a_cnt[0] % 3
            s2 = s3
            a_cnt[0] += 1
            bG = s3
            bU = 3 + s3
            fsl = slice(fc * 128, (fc + 1) * 128)
            ci = e * 8 + fc

            def f():
                for kc in range(8):
                    PE.matmul(PS[:, bG, :], lhsT=Wg_[:, kc, fsl], rhs=h2T[:, kc, gsl], start=(kc == 0), stop=(kc == 7))
                for kc in range(8):
                    ins = PE.matmul(PS[:, bU, :], lhsT=Wu_[:, kc, fsl], rhs=h2T[:, kc, gsl], start=(kc == 0), stop=(kc == 7))
                return ins
            P.op('pe', f, reads=[('W', sg_), ('W', su_)] + [('h2T', kc, g) for kc in range(8)], writes=[('ps', bG), ('ps', bU)])
            P.op('act', lambda: ACT.activation(out=Sg[s2], in_=PS[:, bG, :], func=AF.Sigmoid, bias=bg17[:, ci:ci + 1], scale=1.702),
                 reads=[('ps', bG), 'bg17'], writes=[('Sg', s2)])
            P.op('act', lambda: ACT.activation(out=Gs[s2], in_=PS[:, bG, :], func=AF.Identity, bias=bgc[:, ci:ci + 1], scale=1.0),
                 reads=[('ps', bG), 'bgc'], writes=[('Gs', s2)])
            P.op('act', lambda: ACT.activation(out=Us[s2], in_=PS[:, bU, :], func=AF.Identity, bias=bu1[:, ci:ci + 1], scale=1.0),
                 reads=[('ps', bU), 'bu1'], writes=[('Us', s2)])
            P.op('dve', lambda: V.tensor_tensor(out=Gs[s2], in0=Gs[s2], in1=Sg[s2], op=ALU.mult),
                 reads=[('Sg', s2), ('Gs', s2)], writes=[('Gs', s2)])
            P.op('dve', lambda: V.tensor_scalar(out=Us[s2], in0=Us[s2], scalar1=8.0, scalar2=-6.0, op0=ALU.min, op1=ALU.max),
                 reads=[('Us', s2)], writes=[('Us', s2)])
            P.op('dve', lambda: V.scalar_tensor_tensor(out=hT[:, fc, :], in0=Gs[s2], scalar=7.0 * SIG_MAX, in1=Us[s2], op0=ALU.min, op1=ALU.mult),
                 reads=[('Us', s2), ('Gs', s2)], writes=[('hT', u % 2, fc)])
            uses_left[3 * e] -= 1
            uses_left[3 * e + 1] -= 1
            pump_loads()

        def B_step(u, tt, hh):
            e, g = units[u]
            sd_ = (3 * e + 2) % NSL
            Wd_ = Wsl[sd_]
            hT = hTb[u % 2]
            t = 4 * g + tt
            bY = 6 + (y_cnt[0] % 2)
            y_cnt[0] += 1

            def f():
                for fc in range(8):
                    ins = PE.matmul(PS[:, bY, :], lhsT=hT[:, fc, tt * 128:(tt + 1) * 128], rhs=Wd_[:, fc, hh * 512:(hh + 1) * 512],
                                    start=(fc == 0), stop=(fc == 7))
                return ins
            P.op('pe', f, reads=[('W', sd_)] + [('hT', u % 2, fc) for fc in range(8)], writes=[('ps', bY)])
            P.op('dve', lambda: V.scalar_tensor_tensor(out=Z[:, t, hh * 512:(hh + 1) * 512], in0=PS[:, bY, :], scalar=RW[:, t, e:e + 1],
                                                       in1=Z[:, t, hh * 512:(hh + 1) * 512], op0=ALU.mult, op1=ALU.add),
                 reads=[('ps', bY), ('RW', t), ('Z', t)], writes=[('Z', t)])
            uses_left[3 * e + 2] -= 1
            pump_loads()

        NPRE = 3
        nu = len(units)
        for fc in range(8):
            A_step(0, fc)
        for u in range(nu):
            if u + 1 < nu:
                for fc in range(NPRE):
                    A_step(u + 1, fc)
            for tt in range(4):
                for hh in range(2):
                    B_step(u, tt, hh)
            if u + 1 < nu:
                for fc in range(NPRE, 8):
                    A_step(u + 1, fc)
        phase_end('Wsl0', 'Wsl1', 'Wsl2', 'Wsl3', 'Wsl4', 'ms0', 'ms1', 'hT0', 'hT1', 'Sg0', 'Sg1', 'Sg2', 'Gs0', 'Gs1', 'Gs2', 'Us0', 'Us1', 'Us2', 'h2T')

        lng = A.f32('lng', D)
        lnb = A.f32('lnb', D)
        dma(lng, ln_d[2:3, :].to_broadcast([128, D]), 'lng', writes=['lng'])
        dma(lnb, ln_d[3:4, :].to_broadcast([128, D]), 'lnb', writes=['lnb'])
        ost = [A.f32('ost%d' % i, D) for i in range(2)]
        for t in range(NT):
            s2 = t % 2
            layer_norm_tile(Z[:, t, :], ost[s2], lng, lnb, ('Z', t), ('ost', s2), ['lng', 'lnb'])
            dma(out_d[t * 128:(t + 1) * 128, :], ost[s2], ('ost', s2), reads=[('ost', s2)], writes=[('out', t)])
        P.op('sp', lambda: nc.sync.nop(), reads=[('out', t) for t in range(NT)])
        P.emit(st)
    return nc


def _host_layout(inputs, b):
    f32 = np.float32
    x = np.ascontiguousarray(inputs["x"][b], dtype=f32)
    c = np.asarray(inputs["c"][b], dtype=f32)
    ccol = np.ascontiguousarray(c.reshape(8, 128).T)
    pos = np.ascontiguousarray(np.asarray(inputs["positions"][b]).astype(np.int32).reshape(1, S))
    return x, ccol, pos


def _shared_layout(inputs):
    f32 = np.float32
    sh = {}
    sh["w_ada"] = np.ascontiguousarray(inputs["w_ada"][0], dtype=f32)
    sh["b_ada"] = np.ascontiguousarray(inputs["b_ada"][0].reshape(1, -1), dtype=f32)
    sh["w_in"] = np.ascontiguousarray(inputs["w_in"][0], dtype=f32)
    sh["rmsq"] = np.ascontiguousarray(np.asarray(inputs["rms_q"][0], dtype=f32).reshape(3, 128).T)
    sh["rmskv"] = np.ascontiguousarray(np.asarray(inputs["rms_kv"][0], dtype=f32).reshape(2, 128).T)
    sh["w_uq"] = np.ascontiguousarray(inputs["w_uq"][0], dtype=f32)
    sh["w_ukv"] = np.ascontiguousarray(inputs["w_ukv"][0], dtype=f32)
    rb = np.asarray(inputs["rel_bias"][0], dtype=f32)
    k = np.arange(128)[:, None, None]
    d = np.arange(5)[None, :, None]
    q = np.arange(128)[None, None, :]
    idx = np.clip(128 * d + q - k, -128, 128) + 128
    bt = rb[:, idx]
    sh["biasT"] = np.ascontiguousarray(bt.transpose(1, 0, 2, 3).reshape(128, 8 * 5 * 128))
    cd = 2 * d + q // 64 - k // 64
    sh["maskT"] = np.ascontiguousarray(((cd >= 0) & (cd <= 8)).astype(f32).reshape(128, 5 * 128))
    sh["w_ba"] = np.ascontiguousarray(inputs["w_branch_a"][0], dtype=f32)
    sh["w_bb"] = np.ascontiguousarray(inputs["w_branch_b"][0], dtype=f32)
    sh["w_out"] = np.ascontiguousarray(inputs["w_out"][0], dtype=f32)
    sh["ln"] = np.ascontiguousarray(np.stack([inputs["ln1_g"][0], inputs["ln1_b"][0], inputs["ln2_g"][0], inputs["ln2_b"][0]]).astype(f32))
    sh["w_router"] = np.ascontiguousarray(inputs["w_router"][0], dtype=f32)
    sh["b_router"] = np.ascontiguousarray(inputs["b_router"][0].reshape(1, -1), dtype=f32)
    sh["w_gate"] = np.ascontiguousarray(inputs["w_gate"][0], dtype=f32)
    sh["w_up"] = np.ascontiguousarray(inputs["w_up"][0], dtype=f32)
    sh["w_down"] = np.ascontiguousarray(inputs["w_down"][0], dtype=f32)
    sh["bgcol"] = np.ascontiguousarray(np.asarray(inputs["b_gate"][0], dtype=f32).reshape(NE, 8, 128).transpose(2, 0, 1).reshape(128, NE * 8))
    sh["bucol"] = np.ascontiguousarray(np.asarray(inputs["b_up"][0], dtype=f32).reshape(NE, 8, 128).transpose(2, 0, 1).reshape(128, NE * 8))
    sh["b_down"] = np.ascontiguousarray(inputs["b_down"][0], dtype=f32)
    sh["ident"] = np.eye(128, dtype=f32)
    fr = (np.float32(10000.0) ** (-(np.arange(16, dtype=f32)) / np.float32(16))).astype(f32)
    sh["freq"] = np.ascontiguousarray(fr[np.arange(128) % 16].reshape(128, 1))
    return sh


def kernel(**inputs):
    nb_ = inputs["x"].shape[0]
    nc = build_program("full")
    sh = _shared_layout(inputs)
    in_maps = []
    for b in range(nb_):
        x, ccol, pos = _host_layout(inputs, b)
        m = dict(sh)
        m["x"] = x
        m["ccol"] = ccol
        m["pos"] = pos
        in_maps.append(m)
    res = run_bass_kernel_spmd(nc, in_maps, core_ids=list(range(nb_)))
    return np.stack([np.asarray(r["out"], dtype=np.float32) for r in res.results], axis=0)
```

```python
import math
from contextlib import ExitStack

import numpy as np
import concourse.bass as bass
import concourse.mybir as mybir
from concourse.bass_utils import run_bass_kernel_spmd

F32 = mybir.dt.float32
BF16 = mybir.dt.bfloat16
I32 = mybir.dt.int32
AF = mybir.ActivationFunctionType
ALU = mybir.AluOpType
ENGS = ['pe', 'act', 'dve', 'pool', 'sp']
P5G = 0
P5K = 99
P5M = 'pad'

S = 2048
D = 1024
NT = 16
NG = 4
NE = 32
ALPHA = 2.0 ** 0.25
LN_EPS = 1e-5
RMS_EPS = 1e-6
SIG_MAX = 1.0 / (1.0 + math.exp(-1.702 * 7.0))
TWO_PI = 2.0 * math.pi
CW1 = 6.28125
CW2 = TWO_PI - 6.28125
PI_LO = 3.1415925


class Prog:
    def __init__(self, nc):
        self.nc = nc
        self.ops = []
        self.last_writer = {}
        self.readers = {}
        self.dma_cum = {}

    def op(self, eng, fn, reads=(), writes=(), dma_key=None):
        ps_reads = [k for k in reads if isinstance(k, tuple) and k[0] == 'ps']
        if ps_reads:
            reads = [k for k in reads if k not in ps_reads]
            writes = list(writes) + ps_reads
        idx = len(self.ops)
        deps = set()
        for k in reads:
            w = self.last_writer.get(k)
            if w is not None:
                deps.add(w)
        for k in writes:
            w = self.last_writer.get(k)
            if w is not None:
                deps.add(w)
            for r in self.readers.get(k, ()):
                deps.add(r)
        o = dict(eng=eng, fn=fn, deps=deps, dma_key=dma_key, signal=False)
        if dma_key is not None:
            self.dma_cum[dma_key] = self.dma_cum.get(dma_key, 0) + 16
            o['dma_cum'] = self.dma_cum[dma_key]
        self.ops.append(o)
        for k in reads:
            self.readers.setdefault(k, []).append(idx)
        for k in writes:
            self.last_writer[k] = idx
            self.readers[k] = []
        return idx

    def barrier(self):
        last = {}
        dm = {}
        for i, o in enumerate(self.ops):
            if o['fn'] is None:
                continue
            last[o['eng']] = i
            if o['dma_key'] is not None:
                dm[o['dma_key']] = i
        deps = set(last.values()) | set(dm.values())
        for e in ENGS:
            self.ops.append(dict(eng=e, fn=None, deps=set(deps), dma_key=None, signal=False))

    def emit(self, stack):
        nc = self.nc
        ops = self.ops
        for o in ops:
            for d in o['deps']:
                a = ops[d]
                if a['fn'] is None or a['dma_key'] is not None:
                    continue
                if a['eng'] == 'pe' and o['eng'] == 'pe':
                    continue
                a['signal'] = True
        cnt = {e: 0 for e in ENGS}
        for o in ops:
            if o['fn'] is not None and o['dma_key'] is None and o['signal']:
                cnt[o['eng']] += 1
                o['sig_n'] = cnt[o['eng']]
        sems = {e: stack.enter_context(nc.semaphore("S_" + e)) for e in ['pe', 'act', 'dve', 'pool', 'sp']}
        dsems = {k: stack.enter_context(nc.semaphore("D_%d" % i)) for i, k in enumerate(self.dma_cum)}

        def run(ename, eh):
            waited = {}
            for o in ops:
                if o['eng'] != ename:
                    continue
                need = {}
                for d in sorted(o['deps']):
                    a = ops[d]
                    if a['fn'] is None:
                        continue
                    if a['dma_key'] is not None:
                        key = ('D', a['dma_key'])
                        val = a['dma_cum']
                        sem = dsems[a['dma_key']]
                    else:
                        if a['eng'] == 'pe' and ename == 'pe':
                            continue
                        key = ('S', a['eng'])
                        val = a['sig_n']
                        sem = sems[a['eng']]
                    if key not in need or need[key][0] < val:
                        need[key] = (val, sem)
                for key, (val, sem) in need.items():
                    if waited.get(key, 0) >= val:
                        continue
                    waited[key] = val
                    eh.wait_ge(sem, val)
                if o['fn'] is None:
                    continue
                ins = o['fn']()
                if o['dma_key'] is not None:
                    ins.then_inc(dsems[o['dma_key']], 16)
                elif o['signal']:
                    ins.then_inc(sems[ename], 1)

        with nc.Block() as block:
            @block.tensor
            def _(e):
                run('pe', e)

            @block.scalar
            def _(e):
                run('act', e)

            @block.vector
            def _(e):
                run('dve', e)

            @block.gpsimd
            def _(e):
                run('pool', e)

            @block.sync
            def _(e):
                run('sp', e)


class Arena:
    def __init__(self, tensor, nwords):
        self.t = tensor
        self.n = nwords
        self.live = {}
        self.pending = []

    def _alloc(self, name, words, top=False):
        words = (words + 7) // 8 * 8
        spans = sorted(self.live.values())
        assert name not in self.live, name
        if top:
            pos = self.n
            for (a, b) in reversed(spans):
                if pos - b >= words:
                    break
                pos = min(pos, a)
            pos -= words
            if pos < 0 or any(a < pos + words and pos < b for (a, b) in spans):
                raise RuntimeError("arena OOM(top) for %s (%d words)" % (name, words))
        else:
            pos = 0
            for (a, b) in spans:
                if a - pos >= words:
                    break
                pos = max(pos, b)
            if pos + words > self.n:
                raise RuntimeError("arena OOM for %s (%d words) live=%s" % (name, words, sorted(self.live.items(), key=lambda kv: kv[1])))
        self.live[name] = (pos, pos + words)
        return pos

    def f32(self, name, n, top=False):
        off = self._alloc(name, n, top)
        return self.t[:, off:off + n]

    def bf16(self, name, n, top=False):
        w = (n + 1) // 2
        off = self._alloc(name, w, top)
        return self.t[:, off:off + w].bitcast(BF16)

    def i32(self, name, n):
        off = self._alloc(name, n)
        return self.t[:, off:off + n].bitcast(I32)

    def free(self, *names):
        self.pending.extend(names)

    def commit(self):
        for nme in self.pending:
            del self.live[nme]
        self.pending = []


def build_program(stage="full", n_exp=NE):
    nc = bass.Bass("TRN2", target_bir_lowering=False, dynamic_dma_scratch_size=512)

    def din(name, shape, dt=F32):
        return nc.dram_tensor(name, shape, dt, kind="ExternalInput").ap()

    x_d = din("x", [S, D])
    ccol_d = din("ccol", [128, 8])
    pos_d = din("pos", [1, S], I32)
    wada_d = din("w_ada", [D, 6 * D])
    bada_d = din("b_ada", [1, 6 * D])
    win_d = din("w_in", [D, 4256])
    rmsq_d = din("rmsq", [128, 3])
    rmskv_d = din("rmskv", [128, 2])
    wuq_d = din("w_uq", [384, 768])
    wukv_d = din("w_ukv", [256, 1024])
    biasT_d = din("biasT", [128, 8 * 5 * 128])
    maskT_d = din("maskT", [128, 5 * 128])
    wba_d = din("w_ba", [512, D])
    wbb_d = din("w_bb", [512, D])
    wout_d = din("w_out", [D, D])
    ln_d = din("ln", [4, D])
    wr_d = din("w_router", [D, NE])
    br_d = din("b_router", [1, NE])
    wg_d = wu_d = wd_d = None
    if stage not in ("x1", "p5", "p5a", "p5b", "p5c", "p5d", "p5e"):
        wg_d = din("w_gate", [n_exp, D, D])
        wu_d = din("w_up", [n_exp, D, D])
        wd_d = din("w_down", [n_exp, D, D])
    bgcol_d = din("bgcol", [128, NE * 8])
    bucol_d = din("bucol", [128, NE * 8])
    bdown_d = din("b_down", [NE, D])
    ident_d = din("ident", [128, 128])
    freq_d = din("freq", [128, 1])
    out_d = nc.dram_tensor("out", [S, D], F32, kind="ExternalOutput").ap()

    P = Prog(nc)
    NAR = 55700
    st = ExitStack()
    with st:
        arena_t = st.enter_context(nc.sbuf_tensor("arena", [128, NAR], F32))
        PS = st.enter_context(nc.psum_tensor("PS", [128, 8, 512], F32))
        A = Arena(arena_t, NAR)
        V = nc.vector
        G = nc.gpsimd
        ACT = nc.scalar
        PE = nc.tensor

        bank_rr = [0]

        def nb(lo=0, hi=8):
            b = lo + bank_rr[0] % (hi - lo)
            bank_rr[0] += 1
            return b

        def dma(out, in_, key, reads=(), writes=(), q='sp'):
            if q == 'sp':
                P.op('sp', lambda: nc.sync.dma_start(out=out, in_=in_), reads=list(reads), writes=list(writes), dma_key=key)
            else:
                P.op('act', lambda: nc.scalar.dma_start(out=out, in_=in_), reads=list(reads), writes=list(writes), dma_key=key)

        def phase_end(*free_names):
            A.free(*free_names)
            P.barrier()
            A.commit()

        ident = A.f32('ident', 128)
        dma(ident, ident_d, 'ident', writes=['ident'])
        identb = A.bf16('identb', 128)
        P.op('dve', lambda: V.tensor_copy(out=identb, in_=ident), reads=['ident'], writes=['identb'])
        ones_bf = A.bf16('ones_bf', 128)
        P.op('pool', lambda: G.memset(ones_bf, 1.0), writes=['ones_bf'])
        ones_f = A.f32('ones_f', 128)
        P.op('pool', lambda: G.memset(ones_f, 1.0), writes=['ones_f'])
        modcol = A.f32('modcol', 96)
        onep = A.f32('onep', 16)
        g1bc = A.f32('g1bc', D)
        g2bc = A.f32('g2bc', D)
        ccol = A.f32('ccol', 8)
        sc = A.f32('sc', 8)
        dma(ccol, ccol_d, 'ccol', writes=['ccol'])
        sgc = A.f32('sgc', 8)
        P.op('act', lambda: ACT.activation(out=sgc, in_=ccol, func=AF.Sigmoid), reads=['ccol'], writes=['sgc'])
        P.op('dve', lambda: V.tensor_tensor(out=sc, in0=ccol, in1=sgc, op=ALU.mult), reads=['ccol', 'sgc'], writes=['sc'])

        def col(j):
            return modcol[:, 2 * j:2 * j + 1]

        wst = [A.f32('wst%d' % i, 4096) for i in range(4)]
        modrow = A.f32('modrow', 6 * D)
        badar = A.f32('badar', 6 * D)
        dma(badar[0:1, :], bada_d, 'badar', writes=['badar'])
        for n in range(12):
            s = n % 4
            dma(wst[s].rearrange("p (k f) -> p k f", k=8),
                wada_d[:, n * 512:(n + 1) * 512].rearrange("(k p) f -> p k f", p=128),
                ('wst', s), writes=[('wst', s)], q=('sp' if n % 2 == 0 else 'act'))
            b = nb()

            def f(s=s, b=b):
                for kc in range(8):
                    ins = PE.matmul(PS[0:1, b, :], lhsT=sc[:, kc:kc + 1], rhs=wst[s][:, kc * 512:(kc + 1) * 512],
                                    start=(kc == 0), stop=(kc == 7))
                return ins
            P.op('pe', f, reads=['sc', ('wst', s)], writes=[('ps', b)])
            P.op('dve', lambda n=n, b=b: V.tensor_tensor(out=modrow[0:1, n * 512:(n + 1) * 512], in0=PS[0:1, b, :],
                                                         in1=badar[0:1, n * 512:(n + 1) * 512], op=ALU.add),
                 reads=[('ps', b), 'badar'], writes=['modrow'])
        b = nb()

        def f(b=b):
            for j in range(48):
                ins = PE.matmul(PS[:, b, 2 * j:2 * j + 2], lhsT=modrow[0:1, j * 128:(j + 1) * 128], rhs=ones_f[0:1, 0:2],
                                start=True, stop=True)
            return ins
        P.op('pe', f, reads=['modrow', 'ones_f'], writes=[('ps', b)])
        P.op('dve', lambda b=b: V.tensor_copy(out=modcol, in_=PS[:, b, 0:96]), reads=[('ps', b)], writes=['modcol'])
        P.op('dve', lambda: V.tensor_scalar(out=onep[:, 0:8], in0=modcol[:, 16:32:2], scalar1=1.0, scalar2=None, op0=ALU.add),
             reads=['modcol'], writes=['onep'])
        P.op('dve', lambda: V.tensor_scalar(out=onep[:, 8:16], in0=modcol[:, 64:80:2], scalar1=1.0, scalar2=None, op0=ALU.add),
             reads=['modcol', 'onep'], writes=['onep'])
        for (gb, base) in ((g1bc, 2 * D), (g2bc, 5 * D)):
            for hh in range(2):
                b = nb()
                P.op('pe', lambda b=b, base=base, hh=hh: PE.matmul(PS[:, b, :], lhsT=ones_f[0:1, :],
                                                                   rhs=modrow[0:1, base + hh * 512:base + (hh + 1) * 512],
                                                                   start=True, stop=True),
                     reads=['modrow', 'ones_f'], writes=[('ps', b)])
                P.op('act', lambda b=b, gb=gb, hh=hh: ACT.copy(out=gb[:, hh * 512:(hh + 1) * 512], in_=PS[:, b, :]),
                     reads=[('ps', b)], writes=[('gbc', id(gb), hh)])
        phase_end('wst0', 'wst1', 'wst2', 'wst3', 'modrow', 'badar', 'ccol', 'sc', 'sgc')

        h1T = A.bf16('h1T', 8 * S).rearrange("p (k t) -> p k t", k=8)
        xs = [A.f32('xs%d' % i, 4 * D).rearrange("p (t f) -> p t f", t=4) for i in range(2)]
        for tg in range(NG):
            s = tg % 2
            dma(xs[s], x_d[tg * 512:(tg + 1) * 512, :].rearrange("(t p) f -> p t f", p=128), ('xs', s), writes=[('xs', s)],
                q=('sp' if tg % 2 == 0 else 'act'))
            for kc in range(8):
                b = nb()

                def f(s=s, kc=kc, b=b):
                    for tt in range(4):
                        ins = PE.transpose(PS[:, b, tt * 128:(tt + 1) * 128], xs[s][:, tt, kc * 128:(kc + 1) * 128], ident)
                    return ins
                P.op('pe', f, reads=[('xs', s), 'ident'], writes=[('ps', b)])
                P.op('act', lambda kc=kc, tg=tg, b=b: ACT.activation(out=h1T[:, kc, tg * 512:(tg + 1) * 512], in_=PS[:, b, :],
                                                                     func=AF.Identity, bias=col(kc), scale=onep[:, kc:kc + 1]),
                     reads=[('ps', b), 'modcol', 'onep'], writes=[('h1T', kc, tg)])
        phase_end('xs0', 'xs1')

        STGW = 2048
        stg = [A.f32('stg%d' % i, STGW) for i in range(2)]
        stg_rr = [0]

        def load_stage(src_ap, kch, ncols):
            assert kch * ncols <= STGW
            s = stg_rr[0] % 2
            stg_rr[0] += 1
            view = stg[s][:, 0:kch * ncols].rearrange("p (k f) -> p k f", k=kch)
            dma(view, src_ap.rearrange("(k p) f -> p k f", p=128), ('stg', s), writes=[('stg', s)])
            return s, view

        def load_w_bf16(dst3, src_ap, kch, ncols, wkey, eng='pool'):
            kstep = max(1, STGW // ncols)
            for k0 in range(0, kch, kstep):
                k1 = min(kch, k0 + kstep)
                s, view = load_stage(src_ap[k0 * 128:k1 * 128, :], k1 - k0, ncols)
                if eng == 'pool':
                    P.op('pool', lambda view=view, k0=k0, k1=k1: G.tensor_copy(out=dst3[:, k0:k1, :], in_=view),
                         reads=[('stg', s)], writes=[wkey])
                else:
                    P.op('dve', lambda view=view, k0=k0, k1=k1: V.tensor_copy(out=dst3[:, k0:k1, :], in_=view),
                         reads=[('stg', s)], writes=[wkey])

        qfT = A.bf16('qfT', 8 * S).rearrange("p (h t) -> p h t", h=8)
        kfT = A.bf16('kfT', 8 * S).rearrange("p (h t) -> p h t", h=8)
        vB_flat = A.bf16('vB', NT * 8 * 65 + 8)
        vB = vB_flat[:, 0:NT * 8 * 65].rearrange("p (t h e) -> p t h e", t=NT, h=8)
        P.op('pool', lambda: G.memset(vB_flat, 1.0), writes=['vB'])
        P.op('pool', lambda: G.memset(qfT.rearrange("p h t -> p (h t)"), 0.0), writes=['qfT_init'])

        wcq = A.bf16('wcq', 8 * 384).rearrange("p (k f) -> p k f", k=8)
        wckv = A.bf16('wckv', 8 * 256).rearrange("p (k f) -> p k f", k=8)
        wkr = A.bf16('wkr', 8 * 128).rearrange("p (k f) -> p k f", k=8)
        wkrr = A.bf16('wkrr', 8 * 128).rearrange("p (k f) -> p k f", k=8)
        Wqf = A.bf16('Wqf', 3 * 8 * 128).rearrange("p (k h f) -> p k h f", k=3, h=8)
        Wqr = A.bf16('Wqr', 3 * 8 * 128).rearrange("p (k h f) -> p k h f", k=3, h=8)
        Wkf = A.bf16('Wkf', 2 * 8 * 128).rearrange("p (k h f) -> p k h f", k=2, h=8)
        Wv = A.bf16('Wv', 2 * 512).rearrange("p (k f) -> p k f", k=2)
        rmsq = A.f32('rmsq', 3)
        rmskv = A.f32('rmskv', 2)
        freq = A.f32('freq', 1)
        dma(rmsq, rmsq_d, 'rmsq', writes=['rmsq'])
        dma(rmskv, rmskv_d, 'rmskv', writes=['rmskv'])
        dma(freq, freq_d, 'freq', writes=['freq'])

        load_w_bf16(wcq, win_d[:, 1536:1920], 8, 384, 'wcq')
        load_w_bf16(wckv, win_d[:, 1920:2176], 8, 256, 'wckv')
        P.op('pool', lambda: G.memset(wkr.rearrange("p k f -> p (k f)"), 0.0), writes=['wkr'])
        P.op('pool', lambda: G.memset(wkrr.rearrange("p k f -> p (k f)"), 0.0), writes=['wkrr'])
        s, view = load_stage(win_d[:, 2176:2208], 8, 32)
        P.op('dve', lambda view=view: V.tensor_copy(out=wkr[:, :, 64:96], in_=view), reads=[('stg', s), 'wkr'], writes=['wkr'])
        P.op('dve', lambda view=view: V.tensor_scalar(out=wkrr[:, :, 64:80], in0=view[:, :, 16:32], scalar1=-1.0, scalar2=None, op0=ALU.mult),
             reads=[('stg', s), 'wkrr'], writes=['wkrr'])
        P.op('dve', lambda view=view: V.tensor_copy(out=wkrr[:, :, 80:96], in_=view[:, :, 0:16]), reads=[('stg', s), 'wkrr'], writes=['wkrr'])
        P.op('pool', lambda: G.memset(Wqf.rearrange("p k h f -> p (k h f)"), 0.0), writes=['Wqf'])
        P.op('pool', lambda: G.memset(Wqr.rearrange("p k h f -> p (k h f)"), 0.0), writes=['Wqr'])
        for k in range(3):
            s, view = load_stage(wuq_d[k * 128:(k + 1) * 128, :], 1, 768)
            v4 = view[:, 0, :].rearrange("p (h f) -> p h f", h=8)
            P.op('dve', lambda k=k, v4=v4: V.tensor_scalar(out=Wqf[:, k, :, 0:96], in0=v4, scalar1=rmsq[:, k:k + 1], scalar2=None, op0=ALU.mult),
                 reads=[('stg', s), 'rmsq', 'Wqf'], writes=['Wqf'])
            P.op('dve', lambda k=k, v4=v4: V.tensor_scalar(out=Wqr[:, k, :, 64:80], in0=v4[:, :, 80:96], scalar1=rmsq[:, k:k + 1], scalar2=-1.0,
                                                           op0=ALU.mult, op1=ALU.mult),
                 reads=[('stg', s), 'rmsq', 'Wqr'], writes=['Wqr'])
            P.op('dve', lambda k=k, v4=v4: V.tensor_scalar(out=Wqr[:, k, :, 80:96], in0=v4[:, :, 64:80], scalar1=rmsq[:, k:k + 1], scalar2=None,
                                                           op0=ALU.mult),
                 reads=[('stg', s), 'rmsq', 'Wqr'], writes=['Wqr'])
        P.op('pool', lambda: G.memset(Wkf.rearrange("p k h f -> p (k h f)"), 0.0), writes=['Wkf'])
        for k in range(2):
            s, view = load_stage(wukv_d[k * 128:(k + 1) * 128, :], 1, 1024)
            v4 = view[:, 0, :].rearrange("p (h f) -> p h f", h=8)
            P.op('dve', lambda k=k, v4=v4: V.tensor_scalar(out=Wkf[:, k, :, 0:64], in0=v4[:, :, 0:64], scalar1=rmskv[:, k:k + 1], scalar2=None, op0=ALU.mult),
                 reads=[('stg', s), 'rmskv', 'Wkf'], writes=['Wkf'])
            P.op('dve', lambda k=k, v4=v4: V.tensor_scalar(out=Wv[:, k, :].rearrange("p (h f) -> p h f", h=8), in0=v4[:, :, 64:128],
                                                           scalar1=rmskv[:, k:k + 1], scalar2=None, op0=ALU.mult),
                 reads=[('stg', s), 'rmskv'], writes=['Wv'])

        posi = A.i32('posi', 512)
        ang = A.f32('ang', 512)
        tC = A.f32('tC', 512)
        tS = A.f32('tS', 512)
        tmpA = A.f32('tmpA', 512)
        tmpB = A.f32('tmpB', 512)
        tmpA2 = A.f32('tmpA2', 512)
        tmpB2 = A.f32('tmpB2', 512)
        cqT = A.bf16('cqT', 3 * 512).rearrange("p (k t) -> p k t", k=3)
        ckvT = A.bf16('ckvT', 2 * 512).rearrange("p (k t) -> p k t", k=2)
        sqt = [A.bf16('sqt%d' % i, 512) for i in range(3)]
        rq = A.f32('rq', 512)
        rkv = A.f32('rkv', 512)
        Cq = A.f32('Cq', 512)
        Sq = A.f32('Sq', 512)
        Ck = A.f32('Ck', 512)
        kper = A.bf16('kper', 512)
        rkc = A.f32('rkc', 8)
        P.op('pool', lambda: G.memset(Sq, 0.0), writes=['Sq'])
        P.op('pool', lambda: G.memset(Cq, 0.0), writes=['Cq'])
        P.op('pool', lambda: G.memset(Ck, 0.0), writes=['Ck'])

        for tg in range(NG):
            tsl = slice(tg * 512, (tg + 1) * 512)
            dma(posi, pos_d[0:1, tsl].to_broadcast([128, 512]), 'posi', writes=['posi'])
            P.op('dve', lambda: V.tensor_copy(out=ang, in_=posi), reads=['posi'], writes=['ang'])
            P.op('dve', lambda: V.tensor_scalar(out=ang, in0=ang, scalar1=freq[:, 0:1], scalar2=None, op0=ALU.mult),
                 reads=['ang', 'freq'], writes=['ang'])
            P.op('dve', lambda: V.tensor_scalar(out=posi, in0=ang, scalar1=1.0 / TWO_PI, scalar2=None, op0=ALU.mult),
                 reads=['ang', 'posi'], writes=['posi'])
            P.op('dve', lambda: V.tensor_copy(out=tmpB, in_=posi), reads=['posi'], writes=['tmpB'])
            P.op('dve', lambda: V.scalar_tensor_tensor(out=ang, in0=tmpB, scalar=-CW1, in1=ang, op0=ALU.mult, op1=ALU.add),
                 reads=['tmpB', 'ang'], writes=['ang'])
            P.op('dve', lambda: V.scalar_tensor_tensor(out=ang, in0=tmpB, scalar=-CW2, in1=ang, op0=ALU.mult, op1=ALU.add),
                 reads=['tmpB', 'ang'], writes=['ang'])
            for (tab, shift) in ((tS, 0.0), (tC, math.pi / 2)):
                P.op('dve', lambda tab=tab, shift=shift: V.tensor_scalar(out=tmpA, in0=ang, scalar1=shift, scalar2=None, op0=ALU.add),
                     reads=['ang'], writes=['tmpA'])
                P.op('dve', lambda: V.tensor_scalar(out=tmpB, in0=tmpA, scalar1=math.pi, scalar2=TWO_PI, op0=ALU.is_gt, op1=ALU.mult),
                     reads=['tmpA'], writes=['tmpB'])
                P.op('dve', lambda: V.tensor_tensor(out=tmpA, in0=tmpA, in1=tmpB, op=ALU.subtract), reads=['tmpA', 'tmpB'], writes=['tmpA'])
                P.op('dve', lambda: V.tensor_scalar(out=tmpB, in0=tmpA, scalar1=-math.pi, scalar2=TWO_PI, op0=ALU.is_lt, op1=ALU.mult),
                     reads=['tmpA'], writes=['tmpB'])
                P.op('dve', lambda: V.tensor_tensor(out=tmpA, in0=tmpA, in1=tmpB, op=ALU.add), reads=['tmpA', 'tmpB'], writes=['tmpA'])
                P.op('dve', lambda: V.tensor_scalar(out=tmpA, in0=tmpA, scalar1=PI_LO, scalar2=-PI_LO, op0=ALU.min, op1=ALU.max),
                     reads=['tmpA'], writes=['tmpA'])
                P.op('act', lambda tab=tab: ACT.activation(out=tab, in_=tmpA, func=AF.Sin), reads=['tmpA'], writes=[('tab', id(tab))])

            bq = nb()
            for m in range(3):
                b = nb()

                def f(m=m, b=b, tg=tg):
                    for kc in range(8):
                        ins = PE.matmul(PS[:, b, :], lhsT=wcq[:, kc, m * 128:(m + 1) * 128], rhs=h1T[:, kc, tg * 512:(tg + 1) * 512],
                                        start=(kc == 0), stop=(kc == 7))
                    return ins
                P.op('pe', f, reads=['wcq'] + [('h1T', kc, tg) for kc in range(8)], writes=[('ps', b)])
                P.op('act', lambda m=m, b=b: ACT.copy(out=cqT[:, m, :], in_=PS[:, b, :]), reads=[('ps', b)], writes=[('cqT', m)])
                P.op('act', lambda m=m, b=b: ACT.activation(out=sqt[m], in_=PS[:, b, :], func=AF.Square), reads=[('ps', b)], writes=[('sqt', m)])
                P.op('pe', lambda m=m, bq=bq: PE.matmul(PS[:, bq, :], lhsT=ones_bf, rhs=sqt[m], start=(m == 0), stop=(m == 2)),
                     reads=['ones_bf', ('sqt', m)], writes=[('ps', bq)])
            P.op('act', lambda bq=bq: ACT.activation(out=rq, in_=PS[:, bq, :], func=AF.Sqrt, bias=RMS_EPS, scale=1.0 / 384.0),
                 reads=[('ps', bq)], writes=['rq'])
            P.op('dve', lambda: V.reciprocal(out=rq, in_=rq), reads=['rq'], writes=['rq'])
            P.op('dve', lambda: V.tensor_copy(out=Cq[0:64, :], in_=rq[0:64, :]), reads=['rq'], writes=['Cq'])
            P.op('dve', lambda: V.tensor_tensor(out=Cq[64:96, :], in0=tC[64:96, :], in1=rq[64:96, :], op=ALU.mult),
                 reads=['rq', ('tab', id(tC)), 'Cq'], writes=['Cq'])
            P.op('dve', lambda: V.tensor_tensor(out=Sq[64:96, :], in0=tS[64:96, :], in1=rq[64:96, :], op=ALU.mult),
                 reads=['rq', ('tab', id(tS))], writes=['Sq'])
            for h in range(8):
                ba = nb()
                bb = nb()

                def f(h=h, ba=ba, bb=bb):
                    for m in range(3):
                        PE.matmul(PS[:, ba, :], lhsT=Wqf[:, m, h, :], rhs=cqT[:, m, :], start=(m == 0), stop=(m == 2))
                    for m in range(3):
                        ins = PE.matmul(PS[:, bb, :], lhsT=Wqr[:, m, h, :], rhs=cqT[:, m, :], start=(m == 0), stop=(m == 2))
                    return ins
                P.op('pe', f, reads=['Wqf', 'Wqr'] + [('cqT', m) for m in range(3)], writes=[('ps', ba), ('ps', bb)])
                tA, tB, kA, kB = (tmpA, tmpB, 'tmpA', 'tmpB') if h % 2 == 0 else (tmpA2, tmpB2, 'tmpA2', 'tmpB2')
                P.op('dve', lambda ba=ba, tA=tA: V.tensor_tensor(out=tA, in0=PS[:, ba, :], in1=Cq, op=ALU.mult), reads=[('ps', ba), 'Cq'], writes=[kA])
                P.op('dve', lambda bb=bb, tB=tB: V.tensor_tensor(out=tB, in0=PS[:, bb, :], in1=Sq, op=ALU.mult), reads=[('ps', bb), 'Sq'], writes=[kB])
                P.op('pool', lambda h=h, tsl=tsl, tA=tA, tB=tB: G.tensor_tensor(out=qfT[:, h, tsl], in0=tA, in1=tB, op=ALU.add),
                     reads=[kA, kB, 'qfT_init'], writes=[('qfT', h, tg)])

            bq = nb()
            bcol = nb()
            for m in range(2):
                b = nb()

                def f(m=m, b=b, tg=tg):
                    for kc in range(8):
                        ins = PE.matmul(PS[:, b, :], lhsT=wckv[:, kc, m * 128:(m + 1) * 128], rhs=h1T[:, kc, tg * 512:(tg + 1) * 512],
                                        start=(kc == 0), stop=(kc == 7))
                    return ins
                P.op('pe', f, reads=['wckv'] + [('h1T', kc, tg) for kc in range(8)], writes=[('ps', b)])
                P.op('act', lambda m=m, b=b: ACT.copy(out=ckvT[:, m, :], in_=PS[:, b, :]), reads=[('ps', b)], writes=[('ckvT', m)])
                P.op('act', lambda m=m, b=b: ACT.activation(out=sqt[m], in_=PS[:, b, :], func=AF.Square), reads=[('ps', b)], writes=[('sqt', m)])
                P.op('pe', lambda m=m, bq=bq: PE.matmul(PS[:, bq, :], lhsT=ones_bf, rhs=sqt[m], start=(m == 0), stop=(m == 1)),
                     reads=['ones_bf', ('sqt', m)], writes=[('ps', bq)])
            def f(bcol=bcol):
                first = True
                for tt in range(4):
                    for m in range(2):
                        ins = PE.matmul(PS[:, bcol, 2 * tt:2 * tt + 2], lhsT=sqt[m][:, tt * 128:(tt + 1) * 128], rhs=ones_bf[:, 0:2],
                                        start=first, stop=(tt == 3 and m == 1), skip_group_check=True)
                        first = False
                return ins
            P.op('pe', f, reads=['ones_bf', ('sqt', 0), ('sqt', 1)], writes=[('ps', bcol)])
            P.op('act', lambda bq=bq: ACT.activation(out=rkv, in_=PS[:, bq, :], func=AF.Sqrt, bias=RMS_EPS, scale=1.0 / 256.0),
                 reads=[('ps', bq)], writes=['rkv'])
            P.op('dve', lambda: V.reciprocal(out=rkv, in_=rkv), reads=['rkv'], writes=['rkv'])
            P.op('act', lambda bcol=bcol: ACT.activation(out=rkc, in_=PS[:, bcol, 0:8], func=AF.Sqrt, bias=RMS_EPS, scale=1.0 / 256.0),
                 reads=[('ps', bcol)], writes=['rkc'])
            P.op('dve', lambda: V.reciprocal(out=rkc, in_=rkc), reads=['rkc'], writes=['rkc'])
            P.op('dve', lambda: V.tensor_copy(out=Ck[0:64, :], in_=rkv[0:64, :]), reads=['rkv'], writes=['Ck'])
            ba = nb()
            bb = nb()

            def f(ba=ba, bb=bb, tg=tg):
                for kc in range(8):
                    PE.matmul(PS[:, ba, :], lhsT=wkr[:, kc, :], rhs=h1T[:, kc, tg * 512:(tg + 1) * 512], start=(kc == 0), stop=(kc == 7))
                for kc in range(8):
                    ins = PE.matmul(PS[:, bb, :], lhsT=wkrr[:, kc, :], rhs=h1T[:, kc, tg * 512:(tg + 1) * 512], start=(kc == 0), stop=(kc == 7))
                return ins
            P.op('pe', f, reads=['wkr', 'wkrr'] + [('h1T', kc, tg) for kc in range(8)], writes=[('ps', ba), ('ps', bb)])
            P.op('dve', lambda ba=ba: V.tensor_tensor(out=tmpA[64:96, :], in0=PS[64:96, ba, :], in1=tC[64:96, :], op=ALU.mult),
                 reads=[('ps', ba), ('tab', id(tC))], writes=['tmpA'])
            P.op('dve', lambda bb=bb: V.tensor_tensor(out=tmpB[64:96, :], in0=PS[64:96, bb, :], in1=tS[64:96, :], op=ALU.mult),
                 reads=[('ps', bb), ('tab', id(tS))], writes=['tmpB'])
            P.op('dve', lambda: V.tensor_tensor(out=kper[64:96, :], in0=tmpA[64:96, :], in1=tmpB[64:96, :], op=ALU.add),
                 reads=['tmpA', 'tmpB'], writes=['kper'])
            for h in range(8):
                ba = nb()

                def f(h=h, ba=ba):
                    for m in range(2):
                        ins = PE.matmul(PS[:, ba, :], lhsT=Wkf[:, m, h, :], rhs=ckvT[:, m, :], start=(m == 0), stop=(m == 1))
                    return ins
                P.op('pe', f, reads=['Wkf', ('ckvT', 0), ('ckvT', 1)], writes=[('ps', ba)])
                P.op('dve', lambda h=h, ba=ba, tsl=tsl: V.tensor_tensor(out=kfT[:, h, tsl], in0=PS[:, ba, :], in1=Ck, op=ALU.mult),
                     reads=[('ps', ba), 'Ck'], writes=[('kfT', h, tg)])
                P.op('pool', lambda h=h, tsl=tsl: G.tensor_copy(out=kfT[64:96, h, tsl], in_=kper[64:96, :]),
                     reads=['kper', ('kfT', h, tg)], writes=[('kfT', h, tg)])
            for tt in range(4):
                t = tg * 4 + tt
                ba = nb()

                def f(tt=tt, ba=ba):
                    for m in range(2):
                        ins = PE.matmul(PS[:, ba, :], lhsT=ckvT[:, m, tt * 128:(tt + 1) * 128], rhs=Wv[:, m, :], start=(m == 0), stop=(m == 1))
                    return ins
                P.op('pe', f, reads=['Wv', ('ckvT', 0), ('ckvT', 1)], writes=[('ps', ba)])
                P.op('dve', lambda t=t, tt=tt, ba=ba: V.tensor_scalar(out=vB[:, t, :, 0:64], in0=PS[:, ba, :].rearrange("p (h e) -> p h e", h=8),
                                                                    scalar1=rkc[:, 2 * tt:2 * tt + 1], scalar2=None, op0=ALU.mult),
                     reads=[('ps', ba), 'rkc', 'vB'], writes=[('vB', t)])

        phase_end('wcq', 'wckv', 'wkr', 'wkrr', 'Wqf', 'Wqr', 'Wkf', 'Wv', 'posi', 'ang', 'tC', 'tS', 'tmpA', 'tmpB', 'tmpA2', 'tmpB2',
                  'cqT', 'ckvT', 'sqt0', 'sqt1', 'sqt2', 'rq', 'rkv', 'Cq', 'Sq', 'Ck', 'kper', 'rkc')

        yT_b = A.bf16('yT_b', 4 * S).rearrange("p (k t) -> p k t", k=4)
        wq = A.bf16('wq', 8 * 512).rearrange("p (k f) -> p k f", k=8)
        wk = A.bf16('wk', 8 * 512).rearrange("p (k f) -> p k f", k=8)
        wv = A.bf16('wv', 8 * 512).rearrange("p (k f) -> p k f", k=8)
        load_w_bf16(wq, win_d[:, 0:512], 8, 512, 'wq')
        load_w_bf16(wk, win_d[:, 512:1024], 8, 512, 'wk')
        load_w_bf16(wv, win_d[:, 1024:1536], 8, 512, 'wv')
        ytok = A.bf16('ytok', NT * 512).rearrange("p (t f) -> p t f", t=NT)
        pT = [A.bf16('pT%d' % i, 512) for i in range(4)]
        rc = A.f32('rc', 4)
        SCL_B = 96.0 ** -0.5
        LOOK = 3
        its = []
        for h in range(8):
            for g in range(NG):
                nkt = 4 * g + 4
                for kt in range(nkt):
                    its.append(dict(h=h, g=g, kt=kt, first=(kt == 0), last=(kt == nkt - 1), bo=6 + (h * NG + g) % 2))

        def mla_score(i):
            it = its[i]
            h, g, kt = it['h'], it['g'], it['kt']
            j0 = max(0, kt - 4 * g)
            bs = nb(0, 6)
            sl = i % 4
            it['sl'] = sl
            it['j0'] = j0
            qs = slice(g * 512 + j0 * 128, (g + 1) * 512)
            P.op('pe', lambda: PE.matmul(PS[:, bs, j0 * 128:512], lhsT=kfT[:, h, kt * 128:(kt + 1) * 128], rhs=qfT[:, h, qs], start=True, stop=True),
                 reads=[('kfT', h, kt // 4), ('qfT', h, g)], writes=[('ps', bs)])
            P.op('act', lambda: ACT.activation(out=pT[sl][:, j0 * 128:512], in_=PS[:, bs, j0 * 128:512], func=AF.Exp, scale=SCL_B),
                 reads=[('ps', bs)], writes=[('pT', sl)])
            if kt >= 4 * g:
                P.op('dve', lambda: V.memset(pT[sl][64:128, j0 * 128:j0 * 128 + 64], 0.0), reads=[('pT', sl)], writes=[('pT', sl)])

        def mla_pv(i):
            it = its[i]
            h, g, kt, sl, j0, bo = it['h'], it['g'], it['kt'], it['sl'], it['j0'], it['bo']
            PSO = PS[:, bo, 0:260].rearrange("p (j e) -> p j e", e=65)

            def f():
                fp = it['first']
                for j in range(j0, 4):
                    ins = PE.matmul(PSO[:, j, :], lhsT=pT[sl][:, j * 128:(j + 1) * 128], rhs=vB[:, kt, h, :],
                                    start=fp, stop=(it['last'] and j == 3), skip_group_check=True)
                    fp = False
                return ins
            P.op('pe', f, reads=[('pT', sl), ('vB', kt)], writes=[('ps', bo)])
            if it['last']:
                P.op('dve', lambda: V.reciprocal(out=rc, in_=PSO[:, :, 64]), reads=[('ps', bo)], writes=['rc'])
                for j in range(4):
                    t = 4 * g + j
                    P.op('dve', lambda j=j, t=t: V.tensor_scalar(out=ytok[:, t, h * 64:(h + 1) * 64], in0=PSO[:, j, 0:64],
                                                                 scalar1=rc[:, j:j + 1], scalar2=None, op0=ALU.mult),
                         reads=[('ps', bo), 'rc'], writes=[('ytok', t)])

        for i in range(len(its) + LOOK):
            if i < len(its):
                mla_score(i)
            if i - LOOK >= 0:
                mla_pv(i - LOOK)
        def ytok_to_T(yT):
            for c in range(4):
                for g in range(NG):
                    b = nb()

                    def f(c=c, g=g, b=b):
                        for j in range(4):
                            ins = PE.matmul(PS[:, b, j * 128:(j + 1) * 128], lhsT=ytok[:, 4 * g + j, c * 128:(c + 1) * 128], rhs=identb,
                                            start=True, stop=True)
                        return ins
                    P.op('pe', f, reads=['identb'] + [('ytok', 4 * g + j) for j in range(4)], writes=[('ps', b)])
                    P.op('act', lambda c=c, g=g, b=b: ACT.copy(out=yT[:, c, g * 512:(g + 1) * 512], in_=PS[:, b, :]),
                         reads=[('ps', b)], writes=[('yT', id(yT), c, g)])
        ytok_to_T(yT_b)
        phase_end('qfT', 'kfT', 'vB', 'pT0', 'pT1', 'pT2', 'pT3', 'rmsq', 'rmskv', 'freq')

        qT = A.bf16('qT', 4 * S).rearrange("p (k t) -> p k t", k=4)
        kT = A.bf16('kT', 4 * S).rearrange("p (k t) -> p k t", k=4)
        vA_flat = A.bf16('vA', NT * 8 * 65 + 8)
        vA = vA_flat[:, 0:NT * 8 * 65].rearrange("p (t h e) -> p t h e", t=NT, h=8)
        P.op('pool', lambda: G.memset(vA_flat, 1.0), writes=['vA'])
        maskT = A.f32('maskT', 640)
        expB = A.bf16('expB', 5120).rearrange("p (h d q) -> p h d q", h=8, d=5)
        bTt = [A.f32('bTt%d' % i, 640) for i in range(2)]
        dma(maskT, maskT_d, 'maskT', writes=['maskT'])
        for h in range(8):
            s2 = h % 2
            dma(bTt[s2], biasT_d[:, h * 640:(h + 1) * 640], ('bTt', s2), writes=[('bTt', s2)])
            P.op('act', lambda s2=s2: ACT.activation(out=bTt[s2], in_=bTt[s2], func=AF.Exp), reads=[('bTt', s2)], writes=[('bTt', s2)])
            P.op('dve', lambda h=h, s2=s2: V.tensor_tensor(out=expB[:, h, :, :].rearrange("p d q -> p (d q)"), in0=bTt[s2], in1=maskT, op=ALU.mult),
                 reads=[('bTt', s2), 'maskT'], writes=['expB'])
        for (wt, wkey, dst, dkey) in ((wq, 'wq', qT, 'qT'), (wk, 'wk', kT, 'kT')):
            for c in range(4):
                for g in range(NG):
                    b = nb()

                    def f(wt=wt, c=c, g=g, b=b):
                        for kc in range(8):
                            ins = PE.matmul(PS[:, b, :], lhsT=wt[:, kc, c * 128:(c + 1) * 128], rhs=h1T[:, kc, g * 512:(g + 1) * 512],
                                            start=(kc == 0), stop=(kc == 7))
                        return ins
                    P.op('pe', f, reads=[wkey] + [('h1T', kc, g) for kc in range(8)], writes=[('ps', b)])
                    P.op('act', lambda dst=dst, c=c, g=g, b=b: ACT.copy(out=dst[:, c, g * 512:(g + 1) * 512], in_=PS[:, b, :]),
                         reads=[('ps', b)], writes=[(dkey, c, g)])
        for t in range(NT):
            b = nb()

            def f(t=t, b=b):
                for kc in range(8):
                    ins = PE.matmul(PS[:, b, :], lhsT=h1T[:, kc, t * 128:(t + 1) * 128], rhs=wv[:, kc, :], start=(kc == 0), stop=(kc == 7))
                return ins
            P.op('pe', f, reads=['wv'] + [('h1T', kc, t // 4) for kc in range(8)], writes=[('ps', b)])
            P.op('dve', lambda t=t, b=b: V.tensor_copy(out=vA[:, t, :, 0:64], in_=PS[:, b, :].rearrange("p (h e) -> p h e", h=8)),
                 reads=[('ps', b), 'vA'], writes=[('vA', t)])
        phase_end('wq', 'wk', 'wv', 'bTt0', 'bTt1', 'maskT')
        yT_a = A.bf16('yT_a', 4 * S).rearrange("p (k t) -> p k t", k=4)
        et = [A.f32('et%d' % i, 512) for i in range(3)]
        wga = A.bf16('wga', 8 * D).rearrange("p (k f) -> p k f", k=8)
        wgb = A.bf16('wgb', 8 * D).rearrange("p (k f) -> p k f", k=8)
        load_w_bf16(wga, win_d[:, 2208:3232], 8, D, 'wga')
        load_w_bf16(wgb, win_d[:, 3232:4256], 8, D, 'wgb')
        pTa = [A.bf16('pTa%d' % i, 512) for i in range(4)]
        itsA = []
        for h in range(8):
            for g in range(NG):
                ds = [d for d in range(5) if any(4 * g + j - d >= 0 for j in range(4))]
                for d in ds:
                    itsA.append(dict(h=h, g=g, d=d, first=(d == ds[0]), last=(d == ds[-1]), bo=6 + (h * NG + g) % 2))

        def a_score(i):
            it = itsA[i]
            h, g, d = it['h'], it['g'], it['d']
            c = h // 2
            po = (h % 2) * 64
            js = [j for j in range(4) if 4 * g + j - d >= 0]
            j0 = js[0]
            nj = 4 - j0
            bs = nb(0, 6)
            es = i % 3
            sl = i % 4
            it['js'] = js
            it['sl'] = sl

            def f():
                for j in js:
                    T = 4 * g + j
                    ins = PE.matmul(PS[:, bs, j * 128:(j + 1) * 128], lhsT=kT[po:po + 64, c, (T - d) * 128:(T - d + 1) * 128],
                                    rhs=qT[po:po + 64, c, T * 128:(T + 1) * 128], start=True, stop=True)
                return ins
            kgs = sorted(set((4 * g + j - d) // 4 for j in js))
            P.op('pe', f, reads=[('kT', c, kg) for kg in kgs] + [('qT', c, g)], writes=[('ps', bs)])
            P.op('act', lambda: ACT.activation(out=et[es][:, j0 * 128:512], in_=PS[:, bs, j0 * 128:512], func=AF.Exp, scale=0.125),
                 reads=[('ps', bs)], writes=[('et', es)])
            P.op('dve', lambda: V.tensor_tensor(
                out=pTa[sl][:, j0 * 128:512].rearrange("p (j q) -> p j q", j=nj),
                in0=et[es][:, j0 * 128:512].rearrange("p (j q) -> p j q", j=nj),
                in1=expB[:, h, d, :].unsqueeze(1).to_broadcast([128, nj, 128]), op=ALU.mult),
                reads=[('et', es), 'expB'], writes=[('pTa', sl)])

        def a_pv(i):
            it = itsA[i]
            h, g, d, js, sl, bo = it['h'], it['g'], it['d'], it['js'], it['sl'], it['bo']
            PSO = PS[:, bo, 0:260].rearrange("p (j e) -> p j e", e=65)

            def f():
                fp = it['first']
                for j in js:
                    T = 4 * g + j
                    ins = PE.matmul(PSO[:, j, :], lhsT=pTa[sl][:, j * 128:(j + 1) * 128], rhs=vA[:, T - d, h, :],
                                    start=fp, stop=(it['last'] and j == js[-1]), skip_group_check=True)
                    fp = False
                return ins
            P.op('pe', f, reads=[('pTa', sl)] + [('vA', 4 * g + j - d) for j in js], writes=[('ps', bo)])
            if it['last']:
                P.op('dve', lambda: V.reciprocal(out=rc, in_=PSO[:, :, 64]), reads=[('ps', bo)], writes=['rc'])
                for j in range(4):
                    t = 4 * g + j
                    P.op('dve', lambda j=j, t=t: V.tensor_scalar(out=ytok[:, t, h * 64:(h + 1) * 64], in0=PSO[:, j, 0:64],
                                                                 scalar1=rc[:, j:j + 1], scalar2=None, op0=ALU.mult),
                         reads=[('ps', bo), 'rc'], writes=[('ytok', t)])

        for i in range(len(itsA) + LOOK):
            if i < len(itsA):
                a_score(i)
            if i - LOOK >= 0:
                a_pv(i - LOOK)
        ytok_to_T(yT_a)
        phase_end('qT', 'kT', 'vA', 'expB', 'et0', 'et1', 'et2', 'pTa0', 'pTa1', 'pTa2', 'pTa3', 'ytok', 'rc')

        mT = A.bf16('mT', 8 * S).rearrange("p (k t) -> p k t", k=8)
        wba = A.bf16('wba', 4 * D).rearrange("p (k f) -> p k f", k=4)
        wbb = A.bf16('wbb', 4 * D).rearrange("p (k f) -> p k f", k=4)
        load_w_bf16(wba, wba_d, 4, D, 'wba')
        load_w_bf16(wbb, wbb_d, 4, D, 'wbb')
        sga = [A.f32('sga%d' % i, 512) for i in range(2)]
        sgb = [A.f32('sgb%d' % i, 512) for i in range(2)]
        it = 0
        for fc in range(8):
            for g in range(NG):
                s2 = it % 2
                bset = (it % 2) * 4
                it += 1
                b_a, b_b, b_ga, b_gb = bset, bset + 1, bset + 2, bset + 3
                fsl = slice(fc * 128, (fc + 1) * 128)
                gsl = slice(g * 512, (g + 1) * 512)

                def f(fsl=fsl, gsl=gsl, b_ga=b_ga, b_gb=b_gb, b_a=b_a, b_b=b_b):
                    for kc in range(8):
                        PE.matmul(PS[:, b_ga, :], lhsT=wga[:, kc, fsl], rhs=h1T[:, kc, gsl], start=(kc == 0), stop=(kc == 7))
                    for kc in range(8):
                        PE.matmul(PS[:, b_gb, :], lhsT=wgb[:, kc, fsl], rhs=h1T[:, kc, gsl], start=(kc == 0), stop=(kc == 7))
                    for m in range(4):
                        PE.matmul(PS[:, b_a, :], lhsT=wba[:, m, fsl], rhs=yT_a[:, m, gsl], start=(m == 0), stop=(m == 3))
                    for m in range(4):
                        ins = PE.matmul(PS[:, b_b, :], lhsT=wbb[:, m, fsl], rhs=yT_b[:, m, gsl], start=(m == 0), stop=(m == 3))
                    return ins
                P.op('pe', f, reads=['wga', 'wgb', 'wba', 'wbb'] + [('h1T', kc, g) for kc in range(8)]
                     + [('yT', id(yT_a), m, g) for m in range(4)] + [('yT', id(yT_b), m, g) for m in range(4)],
                     writes=[('ps', b_a), ('ps', b_b), ('ps', b_ga), ('ps', b_gb)])
                P.op('act', lambda s2=s2, b_ga=b_ga: ACT.activation(out=sga[s2], in_=PS[:, b_ga, :], func=AF.Sigmoid), reads=[('ps', b_ga)], writes=[('sga', s2)])
                P.op('act', lambda s2=s2, b_gb=b_gb: ACT.activation(out=sgb[s2], in_=PS[:, b_gb, :], func=AF.Sigmoid), reads=[('ps', b_gb)], writes=[('sgb', s2)])
                P.op('dve', lambda s2=s2, b_a=b_a: V.tensor_tensor(out=sga[s2], in0=PS[:, b_a, :], in1=sga[s2], op=ALU.mult),
                     reads=[('ps', b_a), ('sga', s2)], writes=[('sga', s2)])
                P.op('dve', lambda s2=s2, b_b=b_b: V.tensor_tensor(out=sgb[s2], in0=PS[:, b_b, :], in1=sgb[s2], op=ALU.mult),
                     reads=[('ps', b_b), ('sgb', s2)], writes=[('sgb', s2)])
                P.op('pool', lambda s2=s2, fc=fc, gsl=gsl: G.tensor_tensor(out=mT[:, fc, gsl], in0=sga[s2], in1=sgb[s2], op=ALU.add),
                     reads=[('sga', s2), ('sgb', s2)], writes=[('mT', fc, g)])
        phase_end('wba', 'wbb', 'wga', 'wgb', 'sga0', 'sga1', 'sgb0', 'sgb1', 'h1T', 'yT_a', 'yT_b')

        Z = A.f32('Z', NT * D, top=True).rearrange("p (t f) -> p t f", t=NT)
        wout = A.bf16('wout', 8 * D).rearrange("p (k f) -> p k f", k=8)
        for k0 in range(0, 8, 2):
            s_, view = load_stage(wout_d[k0 * 128:(k0 + 2) * 128, :], 2, D)
            P.op('pool', lambda view=view, k0=k0: G.tensor_tensor(out=wout[:, k0:k0 + 2, :], in0=view,
                                                                  in1=g1bc.unsqueeze(1).to_broadcast([128, 2, D]), op=ALU.mult),
                 reads=[('stg', s_), ('gbc', id(g1bc), 0), ('gbc', id(g1bc), 1)], writes=['wout'])
        lng = A.f32('lng', D)
        lnb = A.f32('lnb', D)
        dma(lng, ln_d[0:1, :].to_broadcast([128, D]), 'lng', writes=['lng'])
        dma(lnb, ln_d[1:2, :].to_broadcast([128, D]), 'lnb', writes=['lnb'])
        xr = [A.f32('xr%d' % i, D) for i in range(2)]
        ztmp = [A.f32('ztmp%d' % i, D) for i in range(2)]
        stats = A.f32('stats', 12)
        mv = A.f32('mv', 2)
        rstd = A.f32('rstd', 1)

        def layer_norm_tile(src, dst, gam, bet, skey, dkey, gkeys):
            P.op('dve', lambda: V.bn_stats(out=stats[:, 0:6], in_=src[:, 0:512]), reads=[skey], writes=['stats'])
            P.op('dve', lambda: V.bn_stats(out=stats[:, 6:12], in_=src[:, 512:1024]), reads=[skey, 'stats'], writes=['stats'])
            P.op('dve', lambda: V.bn_aggr(out=mv, in_=stats), reads=['stats'], writes=['mv'])
            P.op('act', lambda: ACT.activation(out=rstd, in_=mv[:, 1:2], func=AF.Sqrt, bias=LN_EPS, scale=1.0), reads=['mv'], writes=['rstd'])
            P.op('dve', lambda: V.reciprocal(out=rstd, in_=rstd), reads=['rstd'], writes=['rstd'])
            P.op('dve', lambda: V.tensor_scalar(out=src, in0=src, scalar1=mv[:, 0:1], scalar2=rstd[:, 0:1], op0=ALU.subtract, op1=ALU.mult),
                 reads=[skey, 'mv', 'rstd'], writes=[skey])
            P.op('pool', lambda: G.tensor_tensor(out=src, in0=src, in1=gam, op=ALU.mult), reads=[skey] + gkeys, writes=[skey])
            P.op('pool', lambda: G.tensor_tensor(out=dst, in0=src, in1=bet, op=ALU.add), reads=[skey] + gkeys, writes=[dkey])

        for t in range(NT):
            s2 = t % 2
            dma(xr[s2], x_d[t * 128:(t + 1) * 128, :], ('xr', s2), writes=[('xr', s2)])
            for hh in range(2):
                b = nb()

                def f(t=t, hh=hh, b=b):
                    for kc in range(8):
                        ins = PE.matmul(PS[:, b, :], lhsT=mT[:, kc, t * 128:(t + 1) * 128], rhs=wout[:, kc, hh * 512:(hh + 1) * 512],
                                        start=(kc == 0), stop=(kc == 7))
                    return ins
                P.op('pe', f, reads=['wout'] + [('mT', kc, t // 4) for kc in range(8)], writes=[('ps', b)])
                P.op('dve', lambda s2=s2, hh=hh, b=b: V.scalar_tensor_tensor(out=ztmp[s2][:, hh * 512:(hh + 1) * 512], in0=xr[s2][:, hh * 512:(hh + 1) * 512],
                                                                             scalar=ALPHA, in1=PS[:, b, :], op0=ALU.mult, op1=ALU.add),
                     reads=[('ps', b), ('xr', s2), ('ztmp', s2)], writes=[('ztmp', s2)])
            layer_norm_tile(ztmp[s2], Z[:, t, :], lng, lnb, ('ztmp', s2), ('Z', t), ['lng', 'lnb'])

        def finish_with_Z():
            for t in range(NT):
                dma(out_d[t * 128:(t + 1) * 128, :], Z[:, t, :], ('ost', t % 2), reads=[('Z', t)], writes=[('out', t)])
            P.op('sp', lambda: nc.sync.nop(), reads=[('out', t) for t in range(NT)])
            P.emit(st)
            return nc
        if stage == "x1":
            return finish_with_Z()
        phase_end('mT', 'wout', 'xr0', 'xr1', 'g1bc', 'lng', 'lnb', 'stg0', 'stg1', 'ztmp0', 'ztmp1')

        h2T = A.bf16('h2T', 8 * S, top=True).rearrange("p (k t) -> p k t", k=8)
        h2f = A.f32('h2f', 8 * 512).rearrange("p (k t) -> p k t", k=8)
        wr = A.f32('wr', 8 * NE).rearrange("p (k e) -> p k e", k=8)
        dma(wr, wr_d.rearrange("(k p) e -> p k e", p=128), 'wr', writes=['wr'])
        brbc = A.f32('brbc', NE)
        dma(brbc, br_d[0:1, :].to_broadcast([128, NE]), 'brbc', writes=['brbc'])
        RW = A.f32('RW', NT * NE, top=True).rearrange("p (t e) -> p t e", t=NT)
        RWT = A.f32('RWT', 128)
        bdg4 = A.f32('bdg4', 4 * D).rearrange("p (a f) -> p a f", a=4)
        P.op('pool', lambda: G.memset(bdg4.rearrange("p a f -> p (a f)"), 0.0), writes=['bdg4'])
        for a in range(4):
            dma(bdg4[a * NE:(a + 1) * NE, a, :], bdown_d, ('bdg4', a), reads=['bdg4'], writes=[('bdg4d', a)])
        P.op('dve', lambda: V.tensor_tensor(out=bdg4, in0=bdg4, in1=g2bc.unsqueeze(1).to_broadcast([128, 4, D]), op=ALU.mult),
             reads=['bdg4', ('gbc', id(g2bc), 0), ('gbc', id(g2bc), 1)] + [('bdg4d', a) for a in range(4)], writes=['bdg4f'])
        bg17 = A.f32('bg17', NE * 8, top=True)
        bgc = A.f32('bgc', NE * 8, top=True)
        bu1 = A.f32('bu1', NE * 8, top=True)
        dma(bgc, bgcol_d, 'bgc', writes=['bgc'])
        dma(bu1, bucol_d, 'bu1', writes=['bu1'])
        P.op('dve', lambda: V.tensor_scalar(out=bg17, in0=bgc, scalar1=1.702, scalar2=None, op0=ALU.mult), reads=['bgc'], writes=['bg17'])
        P.op('dve', lambda: V.tensor_scalar(out=bu1, in0=bu1, scalar1=1.0, scalar2=None, op0=ALU.add), reads=['bu1'], writes=['bu1'])
        Lg = A.f32('Lg', NE)
        m8 = A.f32('m8', 8)
        nmx = A.f32('nmx', 1)
        ex = A.f32('ex', NE)
        msk = A.f32('msk', NE)
        ssum = A.f32('ssum', 1)
        if stage == "p5a":
            return finish_with_Z()
        if stage == "full":
            NSL = 5
            Wsl = [A.bf16('Wsl%d' % i, 8 * D).rearrange("p (k f) -> p k f", k=8) for i in range(3)] + [None, None]
            ms = [A.f32('ms%d' % i, 512).rearrange("p (k f) -> p k f", k=1) for i in range(2)]
            chunk_rr = [0]
            wsrc = (wg_d, wu_d, wd_d)

            def load_matrix(mi):
                e, which = divmod(mi, 3)
                slot = mi % NSL
                for q in range(16):
                    s = chunk_rr[0] % 2
                    chunk_rr[0] += 1
                    kq, hq = q // 2, q % 2
                    dma(ms[s], wsrc[which][e, kq * 128:(kq + 1) * 128, hq * 512:(hq + 1) * 512].rearrange("(k p) f -> p k f", p=128), ('ms', s), writes=[('ms', s)])
                    if which == 2:
                        P.op('pool', lambda s=s, slot=slot, kq=kq, hq=hq: G.tensor_tensor(out=Wsl[slot][:, kq:kq + 1, hq * 512:(hq + 1) * 512], in0=ms[s],
                                                                                          in1=g2bc[:, hq * 512:(hq + 1) * 512].unsqueeze(1), op=ALU.mult),
                             reads=[('ms', s), ('gbc', id(g2bc), 0), ('gbc', id(g2bc), 1)], writes=[('W', slot)])
                    else:
                        P.op('pool', lambda s=s, slot=slot, kq=kq, hq=hq: G.tensor_copy(out=Wsl[slot][:, kq:kq + 1, hq * 512:(hq + 1) * 512], in_=ms[s]),
                             reads=[('ms', s)], writes=[('W', slot)])

            n_mat = 3 * n_exp
            uses_left = [NG * 8] * n_mat
            next_mat = [0]
            load_limit = [3]

            def pump_loads():
                while next_mat[0] < min(n_mat, load_limit[0]) and (next_mat[0] < NSL or uses_left[next_mat[0] - NSL] == 0):
                    load_matrix(next_mat[0])
                    next_mat[0] += 1

            pump_loads()

        for g in range(NG):
            gsl = slice(g * 512, (g + 1) * 512)
            for kc in range(8):
                b = nb()

                def f(g=g, kc=kc, b=b):
                    for tt in range(4):
                        ins = PE.transpose(PS[:, b, tt * 128:(tt + 1) * 128], Z[:, 4 * g + tt, kc * 128:(kc + 1) * 128], ident)
                    return ins
                if stage == "p5b" and g == P5G and kc > P5K:
                    continue
                if 'p' in P5M or not (stage == "p5b" and g == P5G):
                    P.op('pe', f, reads=['ident'] + [('Z', 4 * g + tt) for tt in range(4)], writes=[('ps', b)])
                if not ('a' in P5M or not (stage == "p5b" and g == P5G)):
                    continue
                P.op('act', lambda kc=kc, gsl=gsl, b=b: ACT.activation(out=h2T[:, kc, gsl], in_=PS[:, b, :], func=AF.Identity,
                                                                       bias=col(24 + kc), scale=onep[:, 8 + kc:9 + kc]),
                     reads=[('ps', b), 'modcol', 'onep'], writes=[('h2T', kc, g)])
                if not ('d' in P5M or not (stage == "p5b" and g == P5G)):
                    continue
                P.op('dve', lambda kc=kc, b=b: V.tensor_scalar(out=h2f[:, kc, :], in0=PS[:, b, :], scalar1=onep[:, 8 + kc:9 + kc], scalar2=col(24 + kc),
                                                               op0=ALU.mult, op1=ALU.add),
                     reads=[('ps', b), 'modcol', 'onep'], writes=[('h2f', kc)])
            if stage == "p5b" and g == P5G:
                return finish_with_Z()
            for tt in range(4):
                t = 4 * g + tt
                P.op('act', lambda t=t: ACT.mul(out=Z[:, t, :], in_=Z[:, t, :], mul=ALPHA), reads=[('Z', t)], writes=[('Z', t)])
                b = nb()

                def f(tt=tt, b=b):
                    for kc in range(8):
                        ins = PE.matmul(PS[:, b, 0:NE], lhsT=h2f[:, kc, tt * 128:(tt + 1) * 128], rhs=wr[:, kc, :], start=(kc == 0), stop=(kc == 7))
                    return ins
                P.op('pe', f, reads=['wr'] + [('h2f', kc) for kc in range(8)], writes=[('ps', b)])
                P.op('dve', lambda b=b: V.tensor_tensor(out=Lg, in0=PS[:, b, 0:NE], in1=brbc, op=ALU.add), reads=[('ps', b), 'brbc'], writes=['Lg'])
                P.op('dve', lambda: V.max(out=m8, in_=Lg), reads=['Lg'], writes=['m8'])
                P.op('dve', lambda: V.tensor_scalar(out=nmx, in0=m8[:, 0:1], scalar1=-1.0, scalar2=None, op0=ALU.mult), reads=['m8'], writes=['nmx'])
                P.op('act', lambda: ACT.activation(out=ex, in_=Lg, func=AF.Exp, bias=nmx[:, 0:1], scale=1.0), reads=['Lg', 'nmx'], writes=['ex'])
                P.op('dve', lambda: V.tensor_scalar(out=msk, in0=Lg, scalar1=m8[:, 3:4], scalar2=None, op0=ALU.is_ge), reads=['Lg', 'm8'], writes=['msk'])
                P.op('dve', lambda: V.tensor_tensor(out=ex, in0=ex, in1=msk, op=ALU.mult), reads=['ex', 'msk'], writes=['ex'])
                P.op('dve', lambda: V.reduce_sum(out=ssum, in_=ex, axis=mybir.AxisListType.X), reads=['ex'], writes=['ssum'])
                P.op('dve', lambda: V.reciprocal(out=ssum, in_=ssum), reads=['ssum'], writes=['ssum'])
                P.op('dve', lambda t=t: V.tensor_scalar(out=RW[:, t, :], in0=ex, scalar1=ssum[:, 0:1], scalar2=None, op0=ALU.mult),
                     reads=['ex', 'ssum'], writes=[('RW', t)])
            if stage == "p5c" and g == P5G:
                return finish_with_Z()
            b2 = nb()
            P.op('pe', lambda g=g, b2=b2: PE.transpose(PS[:, b2, 0:128], RW[:, 4 * g:4 * g + 4, :].rearrange("p t e -> p (t e)"), ident),
                 reads=[('RW', 4 * g + tt) for tt in range(4)] + ['ident'], writes=[('ps', b2)])
            P.op('act', lambda b2=b2: ACT.copy(out=RWT, in_=PS[:, b2, 0:128]), reads=[('ps', b2)], writes=['RWT'])
            if stage == "p5d" and g == P5G:
                return finish_with_Z()
            for tt in range(4):
                t = 4 * g + tt
                for hh in range(2):
                    b3 = nb()
                    P.op('pe', lambda tt=tt, hh=hh, b3=b3: PE.matmul(PS[:, b3, :], lhsT=RWT, rhs=bdg4[:, tt, hh * 512:(hh + 1) * 512],
                                                                    start=True, stop=True),
                         reads=['RWT', 'bdg4f'], writes=[('ps', b3)])
                    P.op('dve', lambda t=t, hh=hh, b3=b3: V.tensor_tensor(out=Z[:, t, hh * 512:(hh + 1) * 512], in0=PS[:, b3, :],
                                                                         in1=Z[:, t, hh * 512:(hh + 1) * 512], op=ALU.add),
                         reads=[('ps', b3), ('Z', t)], writes=[('Z', t)])
            if stage == "p5e" and g == P5G:
                return finish_with_Z()
        if stage == "p5":
            return finish_with_Z()
        phase_end('h2f', 'wr', 'brbc', 'RWT', 'bdg4', 'Lg', 'm8', 'nmx', 'ex', 'msk', 'ssum')

        for i in (3, 4):
            Wsl[i] = A.bf16('Wsl%d' % i, 8 * D).rearrange("p (k f) -> p k f", k=8)
        hTb = [A.bf16('hT%d' % i, 8 * 512).rearrange("p (k t) -> p k t", k=8) for i in range(2)]
        Sg = [A.bf16('Sg%d' % i, 512) for i in range(3)]
        Gs = [A.bf16('Gs%d' % i, 512) for i in range(3)]
        Us = [A.bf16('Us%d' % i, 512) for i in range(3)]
        load_limit[0] = 10 ** 9
        pump_loads()
        units = [(e, g) for e in range(n_exp) for g in range(NG)]
        a_cnt = [0]
        y_cnt = [0]

        def A_step(u, fc):
            e, g = units[u]
            sg_, su_ = (3 * e) % NSL, (3 * e + 1) % NSL
            Wg_, Wu_ = Wsl[sg_], Wsl[su_]
            hT = hTb[u % 2]
            gsl = slice(g * 512, (g + 1) * 512)
            s3 = a_cnt[0] % 3
            s2 = s3
            a_cnt[0] += 1
            bG = s3
            bU = 3 + s3
            fsl = slice(fc * 128, (fc + 1) * 128)
            ci = e * 8 + fc

            def f():
                for kc in range(8):
                    PE.matmul(PS[:, bG, :], lhsT=Wg_[:, kc, fsl], rhs=h2T[:, kc, gsl], start=(kc == 0), stop=(kc == 7))
                for kc in range(8):
                    ins = PE.matmul(PS[:, bU, :], lhsT=Wu_[:, kc, fsl], rhs=h2T[:, kc, gsl], start=(kc == 0), stop=(kc == 7))
                return ins
            P.op('pe', f, reads=[('W', sg_), ('W', su_)] + [('h2T', kc, g) for kc in range(8)], writes=[('ps', bG), ('ps', bU)])
            P.op('act', lambda: ACT.activation(out=Sg[s2], in_=PS[:, bG, :], func=AF.Sigmoid, bias=bg17[:, ci:ci + 1], scale=1.702),
                 reads=[('ps', bG), 'bg17'], writes=[('Sg', s2)])
            P.op('act', lambda: ACT.activation(out=Gs[s2], in_=PS[:, bG, :], func=AF.Identity, bias=bgc[:, ci:ci + 1], scale=1.0),
                 reads=[('ps', bG), 'bgc'], writes=[('Gs', s2)])
            P.op('act', lambda: ACT.activation(out=Us[s2], in_=PS[:, bU, :], func=AF.Identity, bias=bu1[:, ci:ci + 1], scale=1.0),
                 reads=[('ps', bU), 'bu1'], writes=[('Us', s2)])
            P.op('dve', lambda: V.tensor_tensor(out=Gs[s2], in0=Gs[s2], in1=Sg[s2], op=ALU.mult),
                 reads=[('Sg', s2), ('Gs', s2)], writes=[('Gs', s2)])
            P.op('dve', lambda: V.tensor_scalar(out=Us[s2], in0=Us[s2], scalar1=8.0, scalar2=-6.0, op0=ALU.min, op1=ALU.max),
                 reads=[('Us', s2)], writes=[('Us', s2)])
            P.op('dve', lambda: V.scalar_tensor_tensor(out=hT[:, fc, :], in0=Gs[s2], scalar=7.0 * SIG_MAX, in1=Us[s2], op0=ALU.min, op1=ALU.mult),
                 reads=[('Us', s2), ('Gs', s2)], writes=[('hT', u % 2, fc)])
            uses_left[3 * e] -= 1
            uses_left[3 * e + 1] -= 1
            pump_loads()

        def B_step(u, tt, hh):
            e, g = units[u]
            sd_ = (3 * e + 2) % NSL
            Wd_ = Wsl[sd_]
            hT = hTb[u % 2]
            t = 4 * g + tt
            bY = 6 + (y_cnt[0] % 2)
            y_cnt[0] += 1

            def f():
                for fc in range(8):
                    ins = PE.matmul(PS[:, bY, :], lhsT=hT[:, fc, tt * 128:(tt + 1) * 128], rhs=Wd_[:, fc, hh * 512:(hh + 1) * 512],
                                    start=(fc == 0), stop=(fc == 7))
                return ins
            P.op('pe', f, reads=[('W', sd_)] + [('hT', u % 2, fc) for fc in range(8)], writes=[('ps', bY)])
            P.op('dve', lambda: V.scalar_tensor_tensor(out=Z[:, t, hh * 512:(hh + 1) * 512], in0=PS[:, bY, :], scalar=RW[:, t, e:e + 1],
                                                       in1=Z[:, t, hh * 512:(hh + 1) * 512], op0=ALU.mult, op1=ALU.add),
                 reads=[('ps', bY), ('RW', t), ('Z', t)], writes=[('Z', t)])
            uses_left[3 * e + 2] -= 1
            pump_loads()

        NPRE = 3
        nu = len(units)
        for fc in range(8):
            A_step(0, fc)
        for u in range(nu):
            if u + 1 < nu:
                for fc in range(NPRE):
                    A_step(u + 1, fc)
            for tt in range(4):
                for hh in range(2):
                    B_step(u, tt, hh)
            if u + 1 < nu:
                for fc in range(NPRE, 8):
                    A_step(u + 1, fc)
        phase_end('Wsl0', 'Wsl1', 'Wsl2', 'Wsl3', 'Wsl4', 'ms0', 'ms1', 'hT0', 'hT1', 'Sg0', 'Sg1', 'Sg2', 'Gs0', 'Gs1', 'Gs2', 'Us0', 'Us1', 'Us2', 'h2T')

        lng = A.f32('lng', D)
        lnb = A.f32('lnb', D)
        dma(lng, ln_d[2:3, :].to_broadcast([128, D]), 'lng', writes=['lng'])
        dma(lnb, ln_d[3:4, :].to_broadcast([128, D]), 'lnb', writes=['lnb'])
        ost = [A.f32('ost%d' % i, D) for i in range(2)]
        for t in range(NT):
            s2 = t % 2
            layer_norm_tile(Z[:, t, :], ost[s2], lng, lnb, ('Z', t), ('ost', s2), ['lng', 'lnb'])
            dma(out_d[t * 128:(t + 1) * 128, :], ost[s2], ('ost', s2), reads=[('ost', s2)], writes=[('out', t)])
        P.op('sp', lambda: nc.sync.nop(), reads=[('out', t) for t in range(NT)])
        P.emit(st)
    return nc


def _host_layout(inputs, b):
    f32 = np.float32
    x = np.ascontiguousarray(inputs["x"][b], dtype=f32)
    c = np.asarray(inputs["c"][b], dtype=f32)
    ccol = np.ascontiguousarray(c.reshape(8, 128).T)
    pos = np.ascontiguousarray(np.asarray(inputs["positions"][b]).astype(np.int32).reshape(1, S))
    return x, ccol, pos


def _shared_layout(inputs):
    f32 = np.float32
    sh = {}
    sh["w_ada"] = np.ascontiguousarray(inputs["w_ada"][0], dtype=f32)
    sh["b_ada"] = np.ascontiguousarray(inputs["b_ada"][0].reshape(1, -1), dtype=f32)
    sh["w_in"] = np.ascontiguousarray(inputs["w_in"][0], dtype=f32)
    sh["rmsq"] = np.ascontiguousarray(np.asarray(inputs["rms_q"][0], dtype=f32).reshape(3, 128).T)
    sh["rmskv"] = np.ascontiguousarray(np.asarray(inputs["rms_kv"][0], dtype=f32).reshape(2, 128).T)
    sh["w_uq"] = np.ascontiguousarray(inputs["w_uq"][0], dtype=f32)
    sh["w_ukv"] = np.ascontiguousarray(inputs["w_ukv"][0], dtype=f32)
    rb = np.asarray(inputs["rel_bias"][0], dtype=f32)
    k = np.arange(128)[:, None, None]
    d = np.arange(5)[None, :, None]
    q = np.arange(128)[None, None, :]
    idx = np.clip(128 * d + q - k, -128, 128) + 128
    bt = rb[:, idx]
    sh["biasT"] = np.ascontiguousarray(bt.transpose(1, 0, 2, 3).reshape(128, 8 * 5 * 128))
    cd = 2 * d + q // 64 - k // 64
    sh["maskT"] = np.ascontiguousarray(((cd >= 0) & (cd <= 8)).astype(f32).reshape(128, 5 * 128))
    sh["w_ba"] = np.ascontiguousarray(inputs["w_branch_a"][0], dtype=f32)
    sh["w_bb"] = np.ascontiguousarray(inputs["w_branch_b"][0], dtype=f32)
    sh["w_out"] = np.ascontiguousarray(inputs["w_out"][0], dtype=f32)
    sh["ln"] = np.ascontiguousarray(np.stack([inputs["ln1_g"][0], inputs["ln1_b"][0], inputs["ln2_g"][0], inputs["ln2_b"][0]]).astype(f32))
    sh["w_router"] = np.ascontiguousarray(inputs["w_router"][0], dtype=f32)
    sh["b_router"] = np.ascontiguousarray(inputs["b_router"][0].reshape(1, -1), dtype=f32)
    sh["w_gate"] = np.ascontiguousarray(inputs["w_gate"][0], dtype=f32)
    sh["w_up"] = np.ascontiguousarray(inputs["w_up"][0], dtype=f32)
    sh["w_down"] = np.ascontiguousarray(inputs["w_down"][0], dtype=f32)
    sh["bgcol"] = np.ascontiguousarray(np.asarray(inputs["b_gate"][0], dtype=f32).reshape(NE, 8, 128).transpose(2, 0, 1).reshape(128, NE * 8))
    sh["bucol"] = np.ascontiguousarray(np.asarray(inputs["b_up"][0], dtype=f32).reshape(NE, 8, 128).transpose(2, 0, 1).reshape(128, NE * 8))
    sh["b_down"] = np.ascontiguousarray(inputs["b_down"][0], dtype=f32)
    sh["ident"] = np.eye(128, dtype=f32)
    fr = (np.float32(10000.0) ** (-(np.arange(16, dtype=f32)) / np.float32(16))).astype(f32)
    sh["freq"] = np.ascontiguousarray(fr[np.arange(128) % 16].reshape(128, 1))
    return sh


def kernel(**inputs):
    nb_ = inputs["x"].shape[0]
    nc = build_program("full")
    sh = _shared_layout(inputs)
    in_maps = []
    for b in range(nb_):
        x, ccol, pos = _host_layout(inputs, b)
        m = dict(sh)
        m["x"] = x
        m["ccol"] = ccol
        m["pos"] = pos
        in_maps.append(m)
    res = run_bass_kernel_spmd(nc, in_maps, core_ids=list(range(nb_)))
    return np.stack([np.asarray(r["out"], dtype=np.float32) for r in res.results], axis=0)
```
